# Optimizing a Trainium2 kernel written in Bass

```python
import numpy as np
import jax, jax.numpy as jnp
from jax import lax

D_MODEL = 1024
BATCH = 4
SEQ = 8192
DEPTH = 2

GRID_W = 64
CTX_LEN = 256
EPS = 1e-6
ROPE_BASE = 10000.0

RET_HEADS = 4
RET_DK = 64
RET_DV = 64
RET_CHUNK = 128
NA_HEADS = 8
NA_HD = 64
NA_KH = 8
NA_KW = 16
NA_QBLOCK = 128
MLA_HEADS = 4
MLA_NOPE = 64
MLA_ROPE = 32
MLA_VD = 64
MLA_Q_RANK = 384
MLA_KV_RANK = 256
MLA_QBLOCK = 128
N_EXPERTS = 16
N_GROUPS = 4
EXPERTS_PER_GROUP = N_EXPERTS // N_GROUPS
TOP_K = 2
EXPERT_FF = 256

D_MIX = RET_HEADS * RET_DV + NA_HEADS * NA_HD + MLA_HEADS * MLA_VD
PROJ_SIZES = (RET_HEADS * RET_DK, RET_HEADS * RET_DK, RET_HEADS * RET_DV, RET_HEADS * RET_DV,
              NA_HEADS * NA_HD, NA_HEADS * NA_HD, NA_HEADS * NA_HD,
              MLA_Q_RANK, MLA_KV_RANK, MLA_ROPE)
PROJ_SPLITS = tuple(int(v) for v in np.cumsum(PROJ_SIZES)[:-1])
D_PROJ = int(sum(PROJ_SIZES))

kernel_name = "hybrid_parallel_heads_flow_block"


def rms_norm(x, g):
    xf = x.astype(jnp.float32)
    y = xf * lax.rsqrt(jnp.mean(xf * xf, axis=-1, keepdims=True) + EPS)
    return (y * g.astype(jnp.float32)).astype(x.dtype)


def head_norm(o):
    mu = jnp.mean(o, axis=-1, keepdims=True)
    var = jnp.mean(jnp.square(o - mu), axis=-1, keepdims=True)
    return (o - mu) * lax.rsqrt(var + EPS)


def ada_modulation(cvec, w_mod, b_mod):
    m = jax.nn.silu(cvec) @ w_mod + b_mod
    return jnp.split(m[..., None, :], 6, axis=-1)


def to_heads(a, n):
    b, l, w = a.shape
    return a.reshape(b, l, n, w // n)


def rope_1d(x, pos):
    half = x.shape[-1] // 2
    inv = ROPE_BASE ** (-jnp.arange(half, dtype=jnp.float32) / half)
    ang = pos[:, None] * inv[None, :]
    cos = jnp.cos(ang)[:, None, :]
    sin = jnp.sin(ang)[:, None, :]
    xf = x.astype(jnp.float32)
    x1, x2 = xf[..., :half], xf[..., half:]
    return jnp.concatenate([x1 * cos - x2 * sin, x2 * cos + x1 * sin], axis=-1).astype(x.dtype)


def axial_rope(x, pos_r, pos_c):
    h = x.shape[-1] // 2
    return jnp.concatenate([rope_1d(x[..., :h], pos_r), rope_1d(x[..., h:], pos_c)], axis=-1)


def retention_chunkwise(q, k, v, log_gamma, s0, strict):
    b, h, l, dk = q.shape
    dv = v.shape[-1]
    n = l // RET_CHUNK
    qc = q.reshape(b, h, n, RET_CHUNK, dk)
    kc = k.reshape(b, h, n, RET_CHUNK, dk)
    vc = v.reshape(b, h, n, RET_CHUNK, dv)
    pos = jnp.arange(RET_CHUNK, dtype=jnp.float32)
    diff = pos[:, None] - pos[None, :]
    keep = diff > 0 if strict else diff >= 0
    lg = log_gamma[:, None, None]
    dmat = jnp.where(keep[None], jnp.exp(lg * jnp.maximum(diff, 0.0)[None]), 0.0)
    scores = jnp.einsum('bhncd,bhnkd->bhnck', qc, kc) * dmat[None, :, None]
    intra = jnp.einsum('bhnck,bhnke->bhnce', scores, vc)
    k_w = jnp.exp(log_gamma[:, None] * (RET_CHUNK - 1.0 - pos)[None])
    chunk_kv = jnp.einsum('bhnkd,hk,bhnke->nbhde', kc, k_w, vc)
    g_chunk = jnp.exp(log_gamma * RET_CHUNK)[None, :, None, None]

    def step(state, kv):
        return g_chunk * state + kv, state

    s_final, states = lax.scan(step, s0, chunk_kv)
    q_w = jnp.exp(log_gamma[:, None] * (pos + 1.0)[None])
    inter = jnp.einsum('bhncd,hc,nbhde->bhnce', qc, q_w, states)
    return (intra + inter).reshape(b, h, l, dv), s_final


def bidir_retention(q, k, v, lg_f, lg_b, s0_f, s0_b):
    o_f, s_f = retention_chunkwise(q, k, v, lg_f, s0_f, False)
    o_b, s_b = retention_chunkwise(jnp.flip(q, 2), jnp.flip(k, 2), jnp.flip(v, 2), lg_b, s0_b, True)
    return o_f + jnp.flip(o_b, 2), s_f, s_b


def retention_heads(q_cols, k_cols, v_cols, pos):
    q = to_heads(q_cols, RET_HEADS)
    k = to_heads(k_cols, RET_HEADS)
    v = to_heads(v_cols, RET_HEADS)
    if pos is not None:
        q = axial_rope(q, pos[0], pos[1])
        k = axial_rope(k, pos[0], pos[1])
    q = q.astype(jnp.float32).transpose(0, 2, 1, 3) * (RET_DK ** -0.5)
    k = k.astype(jnp.float32).transpose(0, 2, 1, 3)
    v = v.astype(jnp.float32).transpose(0, 2, 1, 3)
    return q, k, v


def retention_output(o, g):
    b, h, l, dv = o.shape
    on = head_norm(o).transpose(0, 2, 1, 3).reshape(b, l, h * dv)
    return on.astype(g.dtype) * jax.nn.silu(g)


def neighbourhood_tables(rows):
    kh = min(NA_KH, rows)
    s = rows * GRID_W
    t = jnp.arange(s)
    r = t // GRID_W
    c = t % GRID_W
    r0 = jnp.clip(r - kh // 2, 0, rows - kh)
    c0 = jnp.clip(c - NA_KW // 2, 0, GRID_W - NA_KW)
    kr = r0[:, None, None] + jnp.arange(kh)[None, :, None]
    kc = c0[:, None, None] + jnp.arange(NA_KW)[None, None, :]
    idx = (kr * GRID_W + kc).reshape(s, kh * NA_KW)
    dr = kr - r[:, None, None]
    dc = kc - c[:, None, None]
    bidx = ((dr + NA_KH - 1) * (2 * NA_KW - 1) + (dc + NA_KW - 1)).reshape(s, kh * NA_KW)
    return idx.astype(jnp.int32), bidx.astype(jnp.int32)


def neighbourhood_attention(q, k, v, k_ctx, v_ctx, rpb, idx, bidx):
    b, s, h, d = q.shape
    nblk = s // NA_QBLOCK
    nk = idx.shape[-1]
    scale = d ** -0.5
    rpb_flat = rpb.reshape(h, -1)
    qb = jnp.moveaxis(q.reshape(b, nblk, NA_QBLOCK, h, d), 1, 0)
    idx_b = idx.reshape(nblk, NA_QBLOCK, nk)
    bidx_b = bidx.reshape(nblk, NA_QBLOCK, nk)

    def block(args):
        qi, ii, bi = args
        kg = k[:, ii]
        vg = v[:, ii]
        s_loc = jnp.einsum('bqhd,bqkhd->bhqk', qi, kg).astype(jnp.float32) * scale
        s_loc = s_loc + rpb_flat[:, bi].astype(jnp.float32)[None]
        s_ctx = jnp.einsum('bqhd,bkhd->bhqk', qi, k_ctx).astype(jnp.float32) * scale
        p = jax.nn.softmax(jnp.concatenate([s_loc, s_ctx], axis=-1), axis=-1).astype(v.dtype)
        return (jnp.einsum('bhqk,bqkhd->bqhd', p[..., :nk], vg)
                + jnp.einsum('bhqk,bkhd->bqhd', p[..., nk:], v_ctx))

    o = lax.map(block, (qb, idx_b, bidx_b))
    return jnp.moveaxis(o, 0, 1).reshape(b, s, h * d)


def dense_attention(q, k, v):
    b, l, h, dq = q.shape
    s = jnp.einsum('bqhd,bkhd->bhqk', q, k).astype(jnp.float32) * (dq ** -0.5)
    p = jax.nn.softmax(s, axis=-1).astype(v.dtype)
    return jnp.einsum('bhqk,bkhd->bqhd', p, v).reshape(b, l, h * v.shape[-1])


def blocked_attention(q, k, v):
    b, s, h, dq = q.shape
    nblk = s // MLA_QBLOCK
    scale = dq ** -0.5
    qb = jnp.moveaxis(q.reshape(b, nblk, MLA_QBLOCK, h, dq), 1, 0)

    def block(qi):
        sc = jnp.einsum('bqhd,bkhd->bhqk', qi, k).astype(jnp.float32) * scale
        p = jax.nn.softmax(sc, axis=-1).astype(v.dtype)
        return jnp.einsum('bhqk,bkhd->bqhd', p, v)

    o = lax.map(block, qb)
    return jnp.moveaxis(o, 0, 1).reshape(b, s, h * v.shape[-1])


def mla_heads(c_q, c_kv, k_pe, q_norm, kv_norm, w_uq, w_ukv, pos):
    b, l, _ = c_q.shape
    q = (rms_norm(c_q, q_norm) @ w_uq).reshape(b, l, MLA_HEADS, MLA_NOPE + MLA_ROPE)
    kv = (rms_norm(c_kv, kv_norm) @ w_ukv).reshape(b, l, MLA_HEADS, MLA_NOPE + MLA_VD)
    q_nope, q_pe = q[..., :MLA_NOPE], q[..., MLA_NOPE:]
    k_nope, v = kv[..., :MLA_NOPE], kv[..., MLA_NOPE:]
    k_pe = k_pe[:, :, None, :]
    if pos is not None:
        q_pe = axial_rope(q_pe, pos[0], pos[1])
        k_pe = axial_rope(k_pe, pos[0], pos[1])
    k_pe = jnp.broadcast_to(k_pe, (b, l, MLA_HEADS, MLA_ROPE))
    q = jnp.concatenate([q_nope, q_pe], axis=-1)
    k = jnp.concatenate([k_nope, k_pe], axis=-1)
    return q, k, v


def moe_ffn(u, router_w, router_b, w1, w3, w2):
    shp = u.shape
    t = u.reshape(-1, shp[-1])
    scores = jax.nn.sigmoid((t @ router_w).astype(jnp.float32))
    sel = scores + router_b.astype(jnp.float32)
    grp_score = jnp.sum(lax.top_k(sel.reshape(-1, N_GROUPS, EXPERTS_PER_GROUP), TOP_K)[0], axis=-1)
    best = jnp.argmax(grp_score, axis=-1)
    in_grp = (jnp.arange(N_EXPERTS) // EXPERTS_PER_GROUP)[None, :] == best[:, None]
    _, top_idx = lax.top_k(jnp.where(in_grp, sel, -jnp.inf), TOP_K)
    top_w = jnp.take_along_axis(scores, top_idx, axis=-1)
    top_w = top_w / jnp.sum(top_w, axis=-1, keepdims=True)
    gates = jnp.sum(jax.nn.one_hot(top_idx, N_EXPERTS, dtype=jnp.float32) * top_w[..., None], axis=1)
    gates = gates.astype(t.dtype)
    y = jnp.zeros_like(t)
    for e in range(N_EXPERTS):
        hdn = jax.nn.silu(t @ w1[e]) * (t @ w3[e])
        y = y + (hdn @ w2[e]) * gates[:, e:e + 1]
    return y.reshape(shp)


def setup_inputs(seed: int = 0) -> dict:
    key = jax.random.key(seed)
    ks = jax.random.split(key, 24)
    f32 = jnp.float32

    def nrm(k, shape, s):
        return jax.random.normal(k, shape, f32) * s

    base_gamma = 1.0 - 2.0 ** (-5.0 - jnp.arange(RET_HEADS, dtype=f32))
    base_logit = jnp.log(base_gamma) - jnp.log1p(-base_gamma)
    return {
        "x": nrm(ks[0], (BATCH, SEQ, D_MODEL), 1.0),
        "c": nrm(ks[1], (BATCH, D_MODEL), 1.0),
        "ctx": nrm(ks[2], (BATCH, CTX_LEN, D_MODEL), 1.0),
        "c_ctx": nrm(ks[3], (D_MODEL,), 1.0),
        "w_mod": nrm(ks[4], (DEPTH, D_MODEL, 6 * D_MODEL), 0.5 * D_MODEL ** -0.5),
        "b_mod": nrm(ks[5], (DEPTH, 6 * D_MODEL), 0.02),
        "norm1_g": 1.0 + nrm(ks[6], (DEPTH, D_MODEL), 0.02),
        "norm2_g": 1.0 + nrm(ks[7], (DEPTH, D_MODEL), 0.02),
        "w_in": nrm(ks[8], (DEPTH, D_MODEL, D_PROJ), D_MODEL ** -0.5),
        "ret_decay_f": base_logit + nrm(ks[9], (DEPTH, RET_HEADS), 0.1),
        "ret_decay_b": base_logit + nrm(ks[10], (DEPTH, RET_HEADS), 0.1),
        "na_rpb": nrm(ks[11], (DEPTH, NA_HEADS, 2 * NA_KH - 1, 2 * NA_KW - 1), 0.1),
        "mla_q_norm": 1.0 + nrm(ks[12], (DEPTH, MLA_Q_RANK), 0.02),
        "mla_kv_norm": 1.0 + nrm(ks[13], (DEPTH, MLA_KV_RANK), 0.02),
        "w_uq": nrm(ks[14], (DEPTH, MLA_Q_RANK, MLA_HEADS * (MLA_NOPE + MLA_ROPE)), MLA_Q_RANK ** -0.5),
        "w_ukv": nrm(ks[15], (DEPTH, MLA_KV_RANK, MLA_HEADS * (MLA_NOPE + MLA_VD)), MLA_KV_RANK ** -0.5),
        "w_out": nrm(ks[16], (DEPTH, D_MIX, D_MODEL), D_MIX ** -0.5),
        "router_w": nrm(ks[17], (D_MODEL, N_EXPERTS), D_MODEL ** -0.5),
        "router_b": nrm(ks[18], (N_EXPERTS,), 0.01),
        "w1": nrm(ks[19], (DEPTH, N_EXPERTS, D_MODEL, EXPERT_FF), D_MODEL ** -0.5),
        "w3": nrm(ks[20], (DEPTH, N_EXPERTS, D_MODEL, EXPERT_FF), D_MODEL ** -0.5),
        "w2": nrm(ks[21], (DEPTH, N_EXPERTS, EXPERT_FF, D_MODEL), EXPERT_FF ** -0.5),
        "final_norm_g": 1.0 + nrm(ks[22], (D_MODEL,), 0.02),
    }


def reference(x, c, ctx, c_ctx, w_mod, b_mod, norm1_g, norm2_g, w_in, ret_decay_f, ret_decay_b,
              na_rpb, mla_q_norm, mla_kv_norm, w_uq, w_ukv, w_out, router_w, router_b, w1, w3, w2,
              final_norm_g):
    b, s, _ = x.shape
    rows = s // GRID_W
    t = jnp.arange(s)
    pos = ((t // GRID_W).astype(jnp.float32), (t % GRID_W).astype(jnp.float32))
    nb_idx, nb_bias_idx = neighbourhood_tables(rows)
    h, hc = x, ctx
    for l in range(DEPTH):
        last = l == DEPTH - 1
        sh1, sc1, g1, sh2, sc2, g2 = ada_modulation(c, w_mod[l], b_mod[l])
        csh1, csc1, cg1, csh2, csc2, cg2 = ada_modulation(c_ctx, w_mod[l], b_mod[l])

        u = rms_norm(h, norm1_g[l]) * (1.0 + sc1) + sh1
        uc = rms_norm(hc, norm1_g[l]) * (1.0 + csc1) + csh1
        p = jnp.split(u @ w_in[l], PROJ_SPLITS, axis=-1)
        pc = jnp.split(uc @ w_in[l], PROJ_SPLITS, axis=-1)

        lg_f = jax.nn.log_sigmoid(ret_decay_f[l].astype(jnp.float32))
        lg_b = jax.nn.log_sigmoid(ret_decay_b[l].astype(jnp.float32))
        rq_c, rk_c, rv_c = retention_heads(pc[0], pc[1], pc[2], None)
        rq, rk, rv = retention_heads(p[0], p[1], p[2], pos)
        zero_state = jnp.zeros((b, RET_HEADS, RET_DK, RET_DV), jnp.float32)
        ro_c, st_f, st_b = bidir_retention(rq_c, rk_c, rv_c, lg_f, lg_b, zero_state, zero_state)
        ro, _, _ = bidir_retention(rq, rk, rv, lg_f, lg_b, st_f, st_b)
        ret_lat = retention_output(ro, p[3])

        nq_c, nk_c, nv_c = to_heads(pc[4], NA_HEADS), to_heads(pc[5], NA_HEADS), to_heads(pc[6], NA_HEADS)
        nq, nk, nv = to_heads(p[4], NA_HEADS), to_heads(p[5], NA_HEADS), to_heads(p[6], NA_HEADS)
        na_lat = neighbourhood_attention(nq, nk, nv, nk_c, nv_c, na_rpb[l], nb_idx, nb_bias_idx)

        mq_c, mk_c, mv_c = mla_heads(pc[7], pc[8], pc[9], mla_q_norm[l], mla_kv_norm[l], w_uq[l], w_ukv[l], None)
        mq, mk, mv = mla_heads(p[7], p[8], p[9], mla_q_norm[l], mla_kv_norm[l], w_uq[l], w_ukv[l], pos)
        mla_lat = blocked_attention(mq, jnp.concatenate([mk, mk_c], axis=1), jnp.concatenate([mv, mv_c], axis=1))

        h = h + g1 * (jnp.concatenate([ret_lat, na_lat, mla_lat], axis=-1) @ w_out[l])
        u2 = rms_norm(h, norm2_g[l]) * (1.0 + sc2) + sh2
        h = h + g2 * moe_ffn(u2, router_w, router_b, w1[l], w3[l], w2[l])

        if not last:
            ret_ctx = retention_output(ro_c, pc[3])
            na_ctx = dense_attention(nq_c, nk_c, nv_c)
            mla_ctx = dense_attention(mq_c, mk_c, mv_c)
            hc = hc + cg1 * (jnp.concatenate([ret_ctx, na_ctx, mla_ctx], axis=-1) @ w_out[l])
            uc2 = rms_norm(hc, norm2_g[l]) * (1.0 + csc2) + csh2
            hc = hc + cg2 * moe_ffn(uc2, router_w, router_b, w1[l], w3[l], w2[l])
    return rms_norm(h, final_norm_g)
```

```python
import numpy as np
from contextlib import ExitStack
import concourse.bass as bass
import concourse.mybir as mybir
from concourse.bass_utils import run_bass_kernel_spmd

F32 = mybir.dt.float32
BF16 = mybir.dt.bfloat16
AF = mybir.ActivationFunctionType
ALU = mybir.AluOpType
AX = mybir.AxisListType

D = 1024
GRID_W = 64
LC = 256
EPS = 1e-6
NE = 16
FF = 256
NCOL = 3232 + 256 + 256 + 32


class Buf:
    __slots__ = ("w", "r")

    def __init__(self):
        self.w = {}
        self.r = {}


class TT:
    __slots__ = ("t", "b")

    def __init__(self, t):
        self.t = t
        self.b = Buf()

    def __getitem__(self, k):
        return self.t[k]


class Eng:
    def __init__(self, name, obj, is_dma):
        self.name = name
        self.obj = obj
        self.is_dma = is_dma
        self.known = {}
        self.sems = []
        self.n = 0


class KB:
    NSLOT = 16

    def __init__(self, nc, es):
        self.nc = nc
        self.es = es
        self.engs = {}
        self.semobj = {}
        self.semval = {}
        self.clock = {}
        for name, obj, is_dma in (("pe", nc.tensor, False), ("act", nc.scalar, False),
                                  ("dve", nc.vector, False), ("pool", nc.gpsimd, False),
                                  ("sp", nc.sync, True), ("pq", nc.gpsimd, True)):
            e = Eng(name, obj, is_dma)
            ns = self.NSLOT if is_dma else 1
            if name == "pq":
                e.exec_name = "pool"
            else:
                e.exec_name = name
            for i in range(ns):
                key = f"{name}{i}"
                s = es.enter_context(nc.semaphore(f"s_{key}"))
                self.semobj[key] = s
                self.semval[key] = 0
                e.sems.append(key)
            self.engs[name] = e
        self.engs["pq"].known = self.engs["pool"].known
        self.ninstr = 0

    def _wait(self, e, key, val):
        if e.known.get(key, 0) >= val:
            return
        e.obj.wait_ge(self.semobj[key], val)
        self.ninstr += 1
        e.known[key] = val
        ck = self.clock.get((key, val))
        if ck:
            for k2, v2 in ck.items():
                if e.known.get(k2, 0) < v2:
                    e.known[k2] = v2

    def op(self, eng, fn, R=(), W=(), WP=()):
        e = self.engs[eng]
        deps = {}
        own = e.sems if not e.is_dma else ()
        for t in R:
            for k, v in t.b.w.items():
                if deps.get(k, 0) < v:
                    deps[k] = v
        for t in W:
            for k, v in t.b.w.items():
                if k in own:
                    continue
                if deps.get(k, 0) < v:
                    deps[k] = v
            for k, v in t.b.r.items():
                if k in own:
                    continue
                if deps.get(k, 0) < v:
                    deps[k] = v
        for t in WP:
            for k, v in t.b.r.items():
                if k in own:
                    continue
                if deps.get(k, 0) < v:
                    deps[k] = v
        if eng == "pe":
            deps.pop("pe0", None)
        if e.is_dma:
            key = e.sems[e.n % self.NSLOT]
            self._wait(e, key, self.semval[key])
        else:
            key = e.sems[0]
        for k, v in deps.items():
            self._wait(e, k, v)
        ins = fn()
        inc = 16 if e.is_dma else 1
        ins.then_inc(self.semobj[key], inc)
        self.semval[key] += inc
        val = self.semval[key]
        e.n += 1
        self.ninstr += 1
        ck = dict(e.known)
        self.clock[(key, val)] = ck
        for t in R:
            t.b.r[key] = val
        for t in W:
            t.b.w = {key: val}
            t.b.r = {}
        for t in WP:
            t.b.w[key] = val
        return ins

    def barrier(self, bufs=()):
        for name in ("pe", "act", "dve", "pool", "sp"):
            e = self.engs[name]
            for key, val in self.semval.items():
                if val > 0:
                    self._wait(e, key, val)
        self.clock.clear()

    def dma(self, out, in_, R=(), W=(), WP=(), q="sp"):
        e = self.engs[q]
        return self.op(q, lambda: e.obj.dma_start(out=out, in_=in_), R, W, WP)

    def mm(self, out, lhsT, rhs, start, stop, R=(), W=(), WP=()):
        nc = self.nc
        return self.op("pe", lambda: nc.tensor.matmul(out, lhsT=lhsT, rhs=rhs, start=start, stop=stop), R, W, WP)

    def tr(self, out, in_, ident, R=(), W=(), WP=()):
        nc = self.nc
        return self.op("pe", lambda: nc.tensor.transpose(out, in_, ident), R, W, WP)

    def act(self, out, in_, func, R=(), W=(), WP=(), **kw):
        nc = self.nc
        return self.op("act", lambda: nc.scalar.activation(out=out, in_=in_, func=func, **kw), R, W, WP)

    def veng(self, eng):
        return self.nc.vector if eng == "dve" else self.nc.gpsimd

    def tt(self, eng, out, in0, in1, op, R=(), W=(), WP=()):
        v = self.veng(eng)
        return self.op(eng, lambda: v.tensor_tensor(out=out, in0=in0, in1=in1, op=op), R, W, WP)

    def ts(self, eng, out, in0, s1, s2, op0, op1=None, R=(), W=(), WP=(), **kw):
        v = self.veng(eng)
        if op1 is None:
            return self.op(eng, lambda: v.tensor_scalar(out=out, in0=in0, scalar1=s1, scalar2=None, op0=op0, **kw), R, W, WP)
        return self.op(eng, lambda: v.tensor_scalar(out=out, in0=in0, scalar1=s1, scalar2=s2, op0=op0, op1=op1, **kw), R, W, WP)

    def stt(self, eng, out, in0, scalar, in1, op0, op1, R=(), W=(), WP=()):
        v = self.veng(eng)
        return self.op(eng, lambda: v.scalar_tensor_tensor(out=out, in0=in0, scalar=scalar, in1=in1, op0=op0, op1=op1), R, W, WP)

    def cp(self, eng, out, in_, R=(), W=(), WP=()):
        if eng == "act":
            nc = self.nc
            return self.op("act", lambda: nc.scalar.copy(out=out, in_=in_), R, W, WP)
        v = self.veng(eng)
        return self.op(eng, lambda: v.tensor_copy(out=out, in_=in_), R, W, WP)

    def memset(self, eng, ap, val, W=(), WP=()):
        v = self.veng(eng)
        return self.op(eng, lambda: v.memset(ap, val), (), W, WP)

    def sb(self, name, shape, dtype):
        self.uid = getattr(self, "uid", 0) + 1
        return TT(self.es.enter_context(self.nc.sbuf_tensor(f"sb{self.uid}_{name}", shape, dtype)))

    def ps(self, name, shape, dtype=F32):
        self.uid = getattr(self, "uid", 0) + 1
        return TT(self.es.enter_context(self.nc.psum_tensor(f"ps{self.uid}_{name}", shape, dtype)))

    def dram(self, name, shape, dtype, kind="Internal"):
        return TT(self.nc.dram_tensor(name, shape, dtype, kind=kind))


class Phase:
    def __init__(self, k):
        self.k = k

    def __enter__(self):
        self.saved = self.k.es
        self.st = ExitStack()
        self.st.__enter__()
        self.k.es = self.st
        return self

    def __exit__(self, *a):
        self.k.barrier()
        self.k.es = self.saved
        return self.st.__exit__(*a)


def _partner(n_half_block):
    nb = 2 * n_half_block
    return np.array([i + n_half_block if i < n_half_block else i - n_half_block for i in range(nb)])


def _rope_tables(L, ndim):
    t = np.arange(L)
    pos = ((t // GRID_W).astype(np.float32), (t % GRID_W).astype(np.float32))
    hb = ndim // 2
    half = hb // 2
    inv = (10000.0 ** (-np.arange(half, dtype=np.float32) / half)).astype(np.float32)
    cos = np.zeros((ndim, L), np.float32)
    sin = np.zeros((ndim, L), np.float32)
    for i in range(ndim):
        blk = i // hb
        ii = i % hb
        j = ii % half
        ang = (pos[blk] * inv[j]).astype(np.float32)
        cos[i] = np.cos(ang)
        sin[i] = np.sin(ang) * (-1.0 if ii < half else 1.0)
    return cos, sin


def _perm_cols(ndim):
    hb = ndim // 2
    p = _partner(hb // 2)
    return np.concatenate([p + b * hb for b in range(2)])


def _na_tables(rpb, L):
    rows = L // GRID_W
    ntq = L // 128
    out = np.empty((5, 128, 8, 5, 128), np.float32)
    reps = [0, 1, 2, ntq - 2, ntq - 1]
    for v, i in enumerate(reps):
        lo = min(max(i - 2, 0), ntq - 5)
        q = i * 128 + np.arange(128)
        qr, qc = q // GRID_W, q % GRID_W
        r0 = np.clip(qr - 4, 0, rows - 8)
        c0 = np.clip(qc - 8, 0, GRID_W - 16)
        for j in range(5):
            kk = (lo + j) * 128 + np.arange(128)
            kr, kc = kk // GRID_W, kk % GRID_W
            valid = ((kr[:, None] >= r0[None]) & (kr[:, None] < r0[None] + 8) &
                     (kc[:, None] >= c0[None]) & (kc[:, None] < c0[None] + 16))
            dr = np.clip(kr[:, None] - qr[None] + 7, 0, 14)
            dc = np.clip(kc[:, None] - qc[None] + 15, 0, 30)
            g = rpb[:, dr, dc]
            out[v, :, :, j, :] = np.where(valid[None], g, np.float32(-100.0)).transpose(1, 0, 2)
    return out


def na_variant(i, ntq):
    if i < 2:
        return i
    if i >= ntq - 2:
        return 5 - (ntq - i)
    return 2


def host_layout(inp, L):
    f32 = np.float32
    sh = {}
    w_in = inp["w_in"]
    p64 = _perm_cols(64)
    p32 = _perm_cols(32)
    rq_rot = np.concatenate([w_in[:, :, 0 + h * 64 + p64] for h in range(4)], axis=-1)
    rk_rot = np.concatenate([w_in[:, :, 256 + h * 64 + p64] for h in range(4)], axis=-1)
    kpe_rot = w_in[:, :, 3200 + p32]
    sh["w_in"] = np.ascontiguousarray(np.concatenate([w_in, rq_rot, rk_rot, kpe_rot], axis=-1))
    w_uq = inp["w_uq"]
    nope = np.concatenate([w_uq[:, :, h * 96:h * 96 + 64] for h in range(4)], axis=-1)
    pe = np.concatenate([w_uq[:, :, h * 96 + 64:h * 96 + 96] for h in range(4)], axis=-1)
    pe_rot = np.concatenate([w_uq[:, :, h * 96 + 64 + p32] for h in range(4)], axis=-1)
    sh["w_uq"] = np.ascontiguousarray(np.concatenate([nope, pe, pe_rot], axis=-1))
    w_ukv = inp["w_ukv"]
    kn = np.concatenate([w_ukv[:, :, h * 128:h * 128 + 64] for h in range(4)], axis=-1)
    vv = np.concatenate([w_ukv[:, :, h * 128 + 64:h * 128 + 128] for h in range(4)], axis=-1)
    sh["w_ukv"] = np.ascontiguousarray(np.concatenate([kn, vv], axis=-1))
    sh["w_mod"] = inp["w_mod"]
    sh["b_mod"] = np.ascontiguousarray(inp["b_mod"].reshape(2, 48, 128).transpose(0, 2, 1))
    sh["n1g"] = np.ascontiguousarray(inp["norm1_g"].reshape(2, 8, 128).transpose(0, 2, 1))
    sh["n2g"] = np.ascontiguousarray(inp["norm2_g"].reshape(2, 8, 128).transpose(0, 2, 1))
    sh["decay"] = np.ascontiguousarray(np.concatenate([inp["ret_decay_f"], inp["ret_decay_b"]], axis=-1))
    sh["natab"] = np.stack([_na_tables(inp["na_rpb"][l], L) for l in range(2)])
    sh["qn"] = np.ascontiguousarray(inp["mla_q_norm"].reshape(2, 3, 128).transpose(0, 2, 1))
    sh["kvn"] = np.ascontiguousarray(inp["mla_kv_norm"].reshape(2, 2, 128).transpose(0, 2, 1))
    sh["w_out"] = inp["w_out"]
    sh["router_w"] = inp["router_w"]
    sh["router_b"] = np.ascontiguousarray(inp["router_b"].reshape(1, 16))
    sh["w1"] = inp["w1"]
    sh["w3"] = inp["w3"]
    sh["w2"] = inp["w2"]
    sh["fng"] = np.ascontiguousarray(inp["final_norm_g"].reshape(1, D))
    c64, s64 = _rope_tables(L, 64)
    sh["rtab"] = np.stack([np.concatenate([c64, c64]), np.concatenate([s64, s64])])
    c32, s32 = _rope_tables(L, 32)
    sh["mtab"] = np.stack([np.tile(c32, (4, 1)), np.tile(s32, (4, 1))])
    for kk in list(sh):
        sh[kk] = np.ascontiguousarray(sh[kk], dtype=f32)
    per = []
    B = inp["x"].shape[0]
    for b in range(B):
        cv = np.stack([inp["c"][b], inp["c_ctx"]], axis=-1)
        cv = cv.reshape(8, 128, 2).transpose(1, 0, 2)
        per.append({"x": np.ascontiguousarray(inp["x"][b], dtype=f32),
                    "ctx": np.ascontiguousarray(inp["ctx"][b], dtype=f32),
                    "cvec": np.ascontiguousarray(cv, dtype=f32)})
    return sh, per


class Prog:
    def __init__(self, L, depth=2, stop_after=None, dbg=(), cut=99):
        self.cut = cut
        self.L = L
        self.LT = L + LC
        self.depth = depth
        self.dbg = set(dbg)
        self.stop_after = stop_after
        nc = bass.Bass("TRN2", target_bir_lowering=False)
        self.nc = nc
        self.root = ExitStack()
        self.root.__enter__()
        self.k = KB(nc, self.root)
        self._decl()
        self._globals()
        self._run()
        self.k.barrier()
        self.root.__exit__(None, None, None)

    def _in(self, name, shape):
        return TT(self.nc.dram_tensor(name, list(shape), F32, kind="ExternalInput").ap())

    def _scr(self, name, shape, dtype):
        kind = "ExternalOutput" if name in self.dbg else "Internal"
        return TT(self.nc.dram_tensor(name, list(shape), dtype, kind=kind).ap())

    def _decl(self):
        L, LT = self.L, self.LT
        i = self._in
        self.x = i("x", (L, D)); self.ctx = i("ctx", (LC, D)); self.cvec = i("cvec", (128, 8, 2))
        self.w_mod = i("w_mod", (2, D, 6 * D)); self.b_mod = i("b_mod", (2, 128, 48))
        self.n1g = i("n1g", (2, 128, 8)); self.n2g = i("n2g", (2, 128, 8))
        self.w_in = i("w_in", (2, D, NCOL)); self.decay = i("decay", (2, 8))
        self.natab = i("natab", (2, 5, 128, 8 * 5 * 128))
        self.qn = i("qn", (2, 128, 3)); self.kvn = i("kvn", (2, 128, 2))
        self.w_uq = i("w_uq", (2, 384, 512)); self.w_ukv = i("w_ukv", (2, 256, 512))
        self.w_out = i("w_out", (2, D, D)); self.router_w = i("router_w", (D, NE))
        self.router_b = i("router_b", (1, NE))
        self.w1 = i("w1", (2, NE, D, FF)); self.w3 = i("w3", (2, NE, D, FF)); self.w2 = i("w2", (2, NE, FF, D))
        self.fng = i("fng", (1, D)); self.rtab = i("rtab", (2, 128, L)); self.mtab = i("mtab", (2, 128, L))
        self.out = TT(self.nc.dram_tensor("out", [L, D], F32, kind="ExternalOutput").ap())
        s = self._scr
        self.RQT = s("RQT", (256, LT), BF16); self.RKT = s("RKT", (256, LT), BF16)
        self.RV = s("RV", (LT, 256), BF16); self.RG = s("RG", (LT, 256), BF16)
        self.NQT = s("NQT", (512, LT), BF16); self.NKT = s("NKT", (512, LT), BF16)
        self.NV = s("NV", (LT, 512), BF16)
        self.MQT = s("MQT", (384, LT), BF16); self.MKT = s("MKT", (384, LT), BF16)
        self.MV = s("MV", (LT, 256), BF16)
        self.MIXT = s("MIXT", (D, LT), BF16)
        self.H1 = s("H1", (L, D), F32); self.HC1 = s("HC1", (LC, D), F32)
        self.HM = s("HM", (LT, D), F32)
        self.EXPB = s("EXPB", (5, 128, 8 * 5 * 128), BF16)
        self.W1BF = s("W1BF", (NE, 128, 8 * FF), BF16); self.W3BF = s("W3BF", (NE, 128, 8 * FF), BF16)
        self.W2BF = s("W2BF", (NE, 128, 2 * D), BF16)

    def _globals(self):
        k, nc = self.k, self.nc
        self.identf = k.sb("identf", [128, 128], F32)
        self.identb = k.sb("identb", [128, 128], BF16)
        self.onesb = k.sb("onesb", [128, 128], BF16)
        self.onesf = k.sb("onesf", [128, 128], F32)
        self.sel = k.sb("sel", [65, 64], F32)
        idf = self.identf
        k.memset("pool", idf[:], 1.0, W=[idf])
        k.op("pool", lambda: nc.gpsimd.affine_select(out=idf[:], in_=idf[:], pattern=[[-1, 128]],
                                                     compare_op=ALU.is_equal, fill=0.0, base=0,
                                                     channel_multiplier=1), R=[idf], W=[idf])
        k.cp("dve", self.identb[:], idf[:], R=[idf], W=[self.identb])
        k.memset("dve", self.onesb[:], 1.0, W=[self.onesb])
        k.memset("dve", self.onesf[:], 1.0, W=[self.onesf])
        k.memset("dve", self.sel[:], 0.0, W=[self.sel])
        k.memset("dve", self.sel[64:65, :], 1.0, WP=[self.sel])
        self.epsb = k.sb("epsb", [128, 1], F32)
        k.memset("dve", self.epsb[:], EPS, W=[self.epsb])
        self.MT = k.sb("MT", [128, 48, 2], F32)
        self.A1 = k.sb("A1", [128, 8, 2], F32); self.B1 = k.sb("B1", [128, 8, 2], F32)
        self.A2 = k.sb("A2", [128, 8, 2], F32); self.B2 = k.sb("B2", [128, 8, 2], F32)
        self.G1B = k.sb("G1B", [128, 2, D], F32); self.G2B = k.sb("G2B", [128, 2, D], F32)
        self.rwf = k.sb("rwf", [128, 8, NE], F32)
        self.rbb = k.sb("rbb", [128, NE], F32)
        self.fngb = k.sb("fngb", [128, D], F32)
        k.dma(self.rwf[:], self.router_w[:, :].rearrange("(kc k) e -> k kc e", k=128), W=[self.rwf])
        k.dma(self.rbb[:], self.router_b[0:1, :].partition_broadcast(128), W=[self.rbb])
        k.dma(self.fngb[:], self.fng[0:1, :].partition_broadcast(128), W=[self.fngb])

    def _run(self):
        if self.stop_after == "glob":
            return
        for l in range(self.depth):
            last = l == self.depth - 1
            hin = self.x if l == 0 else self.H1
            hcin = self.ctx if l == 0 else self.HC1
            self.mod_phase(l)
            if self.stop_after == "mod":
                return
            self.proj_phase(l, hin, hcin)
            if self.stop_after == "proj":
                return
            self.ret_phase(l, not last)
            if self.stop_after == "ret":
                return
            self.na_phase(l, not last)
            if self.stop_after == "na":
                return
            self.mla_phase(l, not last)
            if self.stop_after == "mla":
                return
            self.out_phase(l, hin, hcin, last)
            if self.stop_after == f"out{l}":
                return

    def chk(self, name):
        used = 229376 - self.nc.sbuf_bytes_remaining
        print(f"[sbuf] {name}: used {used} B/partition", flush=True)
        assert used <= 226000, (name, used)

    def load_cast(self, dst_ap, src_ap, n, dstT, rows=128, inner=None):
        k = self.k
        key = id(k.es)
        if getattr(self, "_stg_key", None) != key:
            self._stg_key = key
            self._stg = [k.sb(f"stg{i}", [128, 2048], F32) for i in range(3)]
            self._stg_n = 0
        i = self._stg_n; self._stg_n += 1
        stg = self._stg[i % 3]
        sv = stg[:rows, :n]
        if inner is not None:
            sv = sv.rearrange("p (a b) -> p a b", b=inner)
        k.dma(sv, src_ap, W=[stg])
        k.cp(("pool", "dve", "act")[i % 3] if getattr(self, "_lc_all", True) else "pool", dst_ap, sv, R=[stg], WP=[dstT])

    def mod_phase(self, l):
        k, nc = self.k, self.nc
        with Phase(k):
            ct = k.sb("ct", [128, 8, 2], F32)
            sct = k.sb("sct", [128, 8, 2], F32)
            bm = k.sb("bm", [128, 48], F32)
            g1 = k.sb("n1", [128, 8], F32); g2 = k.sb("n2", [128, 8], F32)
            k.dma(ct[:], self.cvec[:, :, :], W=[ct])
            k.dma(bm[:], self.b_mod[l], W=[bm])
            k.dma(g1[:], self.n1g[l], W=[g1])
            k.dma(g2[:], self.n2g[l], W=[g2])
            k.act(sct[:], ct[:], AF.Silu, R=[ct], W=[sct])
            psm = k.ps("psm", [128, 96])
            wm = [k.sb(f"wm{i}", [128, 8, 512], F32) for i in range(2)]
            for cg in range(12):
                w = wm[cg % 2]
                k.dma(w[:], self.w_mod[l, :, cg * 512:(cg + 1) * 512].rearrange("(kc k) j -> k kc j", k=128),
                      W=[w], q="sp")
                for jc in range(4):
                    ch = cg * 4 + jc
                    for kc in range(8):
                        k.mm(psm[:, ch * 2:ch * 2 + 2], w[:, kc, jc * 128:(jc + 1) * 128], sct[:, kc, :],
                             kc == 0, kc == 7, R=[w, sct], WP=[psm])
            MT = self.MT
            k.tt("dve", MT[:], psm[:, :].rearrange("p (c s) -> p c s", s=2),
                 bm[:].unsqueeze(2).to_broadcast([128, 48, 2]), ALU.add, R=[psm, bm], W=[MT])
            for (A, Bv, g, sc0, sh0) in ((self.A1, self.B1, g1, 8, 0), (self.A2, self.B2, g2, 32, 24)):
                k.stt("dve", A[:], MT[:, sc0:sc0 + 8, :], 1.0, g[:].unsqueeze(2).to_broadcast([128, 8, 2]),
                      ALU.add, ALU.mult, R=[MT, g], W=[A])
                k.cp("dve", Bv[:], MT[:, sh0:sh0 + 8, :], R=[MT], W=[Bv])
            xs = [k.sb(f"xb{i}", [128, 128], F32) for i in range(4)]
            pg = [k.ps(f"pg{i}", [128, 512]) for i in range(2)]
            n = 0
            for (G, c0) in ((self.G1B, 16), (self.G2B, 40)):
                for s in range(2):
                    for half in range(2):
                        p = pg[n % 2]
                        for j in range(4):
                            X = xs[(n * 4 + j) % 4]
                            k.act(X[:], self.onesf[:], AF.Copy, R=[self.onesf, MT], W=[X],
                                  scale=MT[:, c0 + half * 4 + j, s:s + 1])
                            k.mm(p[:, j * 128:(j + 1) * 128], X[:], self.identf[:], True, True,
                                 R=[X, self.identf], WP=[p])
                        k.cp("act", G[:, s, half * 512:(half + 1) * 512], p[:], R=[p], WP=[G])
                        n += 1

    def proj_phase(self, l, hin, hcin):
        k, nc = self.k, self.nc
        L, LT = self.L, self.LT
        with Phase(k):
            WI = k.sb("WI", [128, 8, NCOL], BF16)
            WUQ = k.sb("WUQ", [128, 3, 512], BF16)
            WUKV = k.sb("WUKV", [128, 2, 512], BF16)
            with Phase(k):
                for kc in range(8):
                    for (c0, c1) in ((0, 1888), (1888, NCOL)):
                        self.load_cast(WI[:, kc, c0:c1], self.w_in[l, kc * 128:(kc + 1) * 128, c0:c1], c1 - c0, WI)
                for kc in range(3):
                    self.load_cast(WUQ[:, kc, :], self.w_uq[l, kc * 128:(kc + 1) * 128, :], 512, WUQ)
                for kc in range(2):
                    self.load_cast(WUKV[:, kc, :], self.w_ukv[l, kc * 128:(kc + 1) * 128, :], 512, WUKV)
            qn = k.sb("qn", [128, 3], F32); kvn = k.sb("kvn", [128, 2], F32)
            k.dma(qn[:], self.qn[l], W=[qn]); k.dma(kvn[:], self.kvn[l], W=[kvn])
            hs = [k.sb(f"hs{i}", [128, 4, D], F32) for i in range(2)]
            junk = k.sb("junk", [128, D], BF16)
            ss = k.sb("ss", [128, 4], F32); rs = k.sb("rs", [128, 4], F32)
            xn = k.sb("xn", [128, 4, D], BF16)
            uT = k.sb("uT", [128, 8, 512], BF16)
            tabs = [k.sb(f"tab{i}", [128, 4, 512], F32) for i in range(2)]
            t1 = [k.sb(f"t1_{i}", [128, 512], F32) for i in range(2)]
            t2 = [k.sb(f"t2_{i}", [128, 512], F32) for i in range(2)]
            ob = [k.sb(f"ob{i}", [128, 512], BF16) for i in range(6)]
            sq = [k.sb(f"sq{i}", [128, 512], BF16) for i in range(3)]
            rstd = k.sb("rstd", [128, 512], F32)
            cqn = k.sb("cqn", [128, 3, 512], BF16)
            ckvn = k.sb("ckvn", [128, 2, 512], BF16)
            rvst = [k.sb(f"rvst{i}", [128, 4, 256], BF16) for i in range(1)]
            rgst = [k.sb(f"rgst{i}", [128, 4, 256], BF16) for i in range(1)]
            nvst = [k.sb(f"nvst{i}", [128, 4, 512], BF16) for i in range(1)]
            mvst = [k.sb(f"mvst{i}", [128, 4, 256], BF16) for i in range(1)]
            psT = [k.ps(f"psT{i}", [128, 1024], BF16) for i in range(2)]
            PB = [k.ps(f"pb{i}", [128, 512]) for i in range(6)]
            st = {"pb": 0, "ob": 0, "tile": 0, "ve": 0}
            self.chk("proj")

            def nps():
                p = PB[st["pb"] % 6]; st["pb"] += 1
                return p

            def nob():
                o = ob[st["ob"] % 6]; st["ob"] += 1
                return o

            def ve():
                st["ve"] += 1
                return "dve" if st["ve"] % 2 else "pool"

            def tile(src_rows, tok0, TW, sidx):
                NS = TW // 128
                ti = st["tile"]; st["tile"] += 1
                lat = sidx == 0
                cols = slice(tok0, tok0 + TW)
                h = hs[ti % 2]
                tb = tabs[ti % 2]

                def loads(j):
                    src_j, tok_j, TW_j, sidx_j = tlist[j]
                    h_j, tb_j = hs[j % 2], tabs[j % 2]
                    k.dma(h_j[:, :TW_j // 128, :], src_j.rearrange("(s p) d -> p s d", p=128), W=[h_j])
                    if sidx_j == 0:
                        k.dma(tb_j[:, 0:2, :TW_j], self.rtab[:, :, tok_j:tok_j + TW_j].rearrange("a p t -> p a t"), WP=[tb_j])
                        k.dma(tb_j[:, 2:4, :TW_j], self.mtab[:, :, tok_j:tok_j + TW_j].rearrange("a p t -> p a t"), WP=[tb_j])

                if ti == 0:
                    loads(0)
                if ti + 1 < len(tlist):
                    loads(ti + 1)
                if self.cut <= 1:
                    return
                for s in range(NS):
                    k.act(junk[:], h[:, s, :], AF.Square, R=[h], W=[junk], WP=[ss], accum_out=ss[:, s:s + 1])
                k.act(rs[:, :NS], ss[:, :NS], AF.Sqrt, R=[ss], W=[rs], scale=1.0 / D, bias=self.epsb[:, 0:1])
                k.op("dve", lambda: nc.vector.reciprocal(out=rs[:, :NS], in_=rs[:, :NS]), R=[rs], W=[rs])
                for s in range(NS):
                    k.act(xn[:, s, :], h[:, s, :], AF.Copy, R=[h, rs], WP=[xn], scale=rs[:, s:s + 1])
                if self.cut <= 2:
                    return
                for j in range(8):
                    pt = psT[j % 2]
                    for s in range(NS):
                        k.tr(pt[:, s * 128:(s + 1) * 128], xn[:, s, j * 128:(j + 1) * 128], self.identb[:],
                             R=[xn, self.identb], WP=[pt])
                    k.act(uT[:, j, :TW], pt[:, :TW], AF.Identity, R=[pt, self.A1, self.B1], WP=[uT],
                          scale=self.A1[:, j, sidx:sidx + 1], bias=self.B1[:, j, sidx:sidx + 1])

                def fm(cs, ncol, W_=WI, nk=8, rhs=uT):
                    p = nps()
                    for kc in range(nk):
                        k.mm(p[:ncol, :TW], W_[:, kc, cs:cs + ncol], rhs[:, kc, :TW], kc == 0, kc == nk - 1,
                             R=[W_, rhs], WP=[p])
                    return p

                def rope(pa, pb_, ncol, ci, si):
                    o = nob()
                    if lat:
                        a = t1[st["ob"] % 2]; b = t2[st["ob"] % 2]
                        k.tt("dve", a[:ncol, :TW], pa[:ncol, :TW], tb[:ncol, ci, :TW], ALU.mult, R=[pa, tb], W=[a])
                        k.tt("dve", b[:ncol, :TW], pb_[:ncol, :TW], tb[:ncol, si, :TW], ALU.mult, R=[pb_, tb], W=[b])
                        k.tt("pool", o[:ncol, :TW], a[:ncol, :TW], b[:ncol, :TW], ALU.add, R=[a, b], W=[o])
                    else:
                        k.cp("act", o[:ncol, :TW], pa[:ncol, :TW], R=[pa], W=[o])
                    return o

                if self.cut <= 3:
                    return
                for (base, rbase, dst) in ((0, 3232, self.RQT), (256, 3488, self.RKT)):
                    for hp in range(2):
                        pa = fm(base + hp * 128, 128)
                        pb_ = fm(rbase + hp * 128, 128) if lat else None
                        o = rope(pa, pb_, 128, 0, 1)
                        k.dma(dst[hp * 128:(hp + 1) * 128, cols], o[:, :TW], R=[o], WP=[dst])
                if self.cut <= 4:
                    return
                for (base, dst) in ((1024, self.NQT), (1536, self.NKT)):
                    for c in range(4):
                        pa = fm(base + c * 128, 128)
                        o = nob()
                        k.cp("act" if c % 2 else "dve", o[:, :TW], pa[:, :TW], R=[pa], W=[o])
                        k.dma(dst[c * 128:(c + 1) * 128, cols], o[:, :TW], R=[o], WP=[dst])
                if self.cut <= 5:
                    return
                rv_, rg_, nv_, mv_ = rvst[0], rgst[0], nvst[0], mvst[0]
                for s in range(NS):
                    import os
                    T = os.environ.get("TOG", "")
                    p = nps()
                    for kc in range(8):
                        if "g" in T:
                            break
                        k.mm(p[:, :], uT[:, kc, s * 128:(s + 1) * 128], WI[:, kc, 512:1024], kc == 0, kc == 7,
                             R=[WI, uT], WP=[p])
                    if "a" not in T:
                        k.cp("act", rv_[:, s, :], p[:, 0:256], R=[p], WP=[rv_])
                    if "b" not in T:
                        k.act(rg_[:, s, :], p[:, 256:512], AF.Silu, R=[p], WP=[rg_])
                    if "c" in T:
                        continue
                    p = nps()
                    for kc in range(8):
                        cofs = 512 if "f" in T else 2048
                        k.mm(p[:, :], uT[:, kc, s * 128:(s + 1) * 128], WI[:, kc, cofs:cofs + 512], kc == 0, kc == 7,
                             R=[WI, uT], WP=[p])
                    if "d" in T:
                        continue
                    k.cp("act" if "e" in T else "dve", nv_[:, s, :], p[:, :], R=[p], WP=[nv_])
                rows = slice(tok0, tok0 + TW)
                if self.cut <= 5.05:
                    return
                k.dma(self.RV[rows, :].rearrange("(s p) c -> p s c", p=128), rv_[:, :NS, :], R=[rv_], WP=[self.RV])
                if self.cut <= 5.1:
                    return
                k.dma(self.RG[rows, :].rearrange("(s p) c -> p s c", p=128), rg_[:, :NS, :], R=[rg_], WP=[self.RG])
                if self.cut <= 5.2:
                    return
                k.dma(self.NV[rows, :].rearrange("(s p) c -> p s c", p=128),
                      nv_[:, :NS, :], R=[nv_], WP=[self.NV])
                if self.cut <= 6:
                    return
                pcs = [fm(2560 + c * 128, 128) for c in range(3)]
                for c in range(3):
                    k.act(sq[c][:, :TW], pcs[c][:, :TW], AF.Square, R=[pcs[c]], W=[sq[c]])
                pss = nps()
                for c in range(3):
                    k.mm(pss[:, :TW], self.onesb[:], sq[c][:, :TW], c == 0, c == 2, R=[self.onesb, sq[c]], WP=[pss])
                k.act(rstd[:, :TW], pss[:, :TW], AF.Sqrt, R=[pss], W=[rstd], scale=1.0 / 384, bias=self.epsb[:, 0:1])
                k.op("dve", lambda: nc.vector.reciprocal(out=rstd[:, :TW], in_=rstd[:, :TW]), R=[rstd], W=[rstd])
                for c in range(3):
                    k.stt("dve", cqn[:, c, :TW], pcs[c][:, :TW], qn[:, c:c + 1], rstd[:, :TW], ALU.mult, ALU.mult,
                          R=[pcs[c], qn, rstd], WP=[cqn])
                for c in range(2):
                    pa = fm(c * 128, 128, WUQ, 3, cqn)
                    o = nob()
                    k.cp("act", o[:, :TW], pa[:, :TW], R=[pa], W=[o])
                    for hh in range(2):
                        hd = 2 * c + hh
                        k.dma(self.MQT[hd * 96:hd * 96 + 64, cols], o[hh * 64:(hh + 1) * 64, :TW], R=[o], WP=[self.MQT])
                pa = fm(256, 128, WUQ, 3, cqn)
                pb_ = fm(384, 128, WUQ, 3, cqn) if lat else None
                o = rope(pa, pb_, 128, 2, 3)
                for hd in range(4):
                    k.dma(self.MQT[hd * 96 + 64:hd * 96 + 96, cols], o[hd * 32:(hd + 1) * 32, :TW], R=[o], WP=[self.MQT])
                if self.cut <= 7:
                    return
                pcs = [fm(2944 + c * 128, 128) for c in range(2)]
                for c in range(2):
                    k.act(sq[c][:, :TW], pcs[c][:, :TW], AF.Square, R=[pcs[c]], W=[sq[c]])
                pss = nps()
                for c in range(2):
                    k.mm(pss[:, :TW], self.onesb[:], sq[c][:, :TW], c == 0, c == 1, R=[self.onesb, sq[c]], WP=[pss])
                k.act(rstd[:, :TW], pss[:, :TW], AF.Sqrt, R=[pss], W=[rstd], scale=1.0 / 256, bias=self.epsb[:, 0:1])
                k.op("dve", lambda: nc.vector.reciprocal(out=rstd[:, :TW], in_=rstd[:, :TW]), R=[rstd], W=[rstd])
                for c in range(2):
                    k.stt("dve", ckvn[:, c, :TW], pcs[c][:, :TW], kvn[:, c:c + 1], rstd[:, :TW], ALU.mult, ALU.mult,
                          R=[pcs[c], kvn, rstd], WP=[ckvn])
                for c in range(2):
                    pa = fm(c * 128, 128, WUKV, 2, ckvn)
                    o = nob()
                    k.cp("act", o[:, :TW], pa[:, :TW], R=[pa], W=[o])
                    for hh in range(2):
                        hd = 2 * c + hh
                        k.dma(self.MKT[hd * 96:hd * 96 + 64, cols], o[hh * 64:(hh + 1) * 64, :TW], R=[o], WP=[self.MKT])
                for s in range(NS):
                    p = nps()
                    for kc in range(2):
                        k.mm(p[:, 0:256], ckvn[:, kc, s * 128:(s + 1) * 128], WUKV[:, kc, 256:512], kc == 0, kc == 1,
                             R=[WUKV, ckvn], WP=[p])
                    k.cp("dve", mv_[:, s, :], p[:, 0:256], R=[p], WP=[mv_])
                k.dma(self.MV[rows, :].rearrange("(s p) c -> p s c", p=128),
                      mv_[:, :NS, :], R=[mv_], WP=[self.MV])
                if self.cut <= 8:
                    return
                pa = fm(3200, 32)
                pb_ = fm(3744, 32) if lat else None
                o = rope(pa, pb_, 32, 2, 3)
                for hd in range(4):
                    k.dma(self.MKT[hd * 96 + 64:hd * 96 + 96, cols], o[0:32, :TW], R=[o], WP=[self.MKT])

            tlist = [(hcin[0:LC, :], L, LC, 1)] + [(hin[t * 512:(t + 1) * 512, :], t * 512, 512, 0)
                                                    for t in range(L // 512)]
            for tl in tlist:
                tile(*tl)

    def ret_phase(self, l, do_ctx):
        k, nc = self.k, self.nc
        L, LT = self.L, self.LT
        NL = L // 128
        NCH = LT // 128
        c0x, c1x = NL, NL + 1
        order_f = [c0x, c1x] + list(range(NL))
        order_b = [c1x, c0x] + list(range(NL - 1, -1, -1))
        with Phase(k):
            dec = k.sb("rdec", [128, 8], F32)
            lg = k.sb("rlg", [128, 8], F32)
            k.dma(dec[:], self.decay[l:l + 1, :].partition_broadcast(128), W=[dec])
            k.act(lg[:], dec[:], AF.Exp, R=[dec], W=[lg], scale=-1.0)
            k.ts("dve", lg[:], lg[:], 1.0, None, ALU.add, R=[lg], W=[lg])
            k.act(lg[:], lg[:], AF.Ln, R=[lg], W=[lg])
            k.ts("dve", lg[:], lg[:], -1.0, None, ALU.mult, R=[lg], W=[lg])
            Dm = k.sb("rD", [128, 128], F32)
            k.op("pool", lambda: nc.gpsimd.iota(Dm[:], pattern=[[1, 128]], base=0, channel_multiplier=-1,
                                                allow_small_or_imprecise_dtypes=True), W=[Dm])
            Dpos = k.sb("rDp", [128, 128], F32); Dneg = k.sb("rDn", [128, 128], F32)
            mge = k.sb("rmge", [128, 128], F32); mlt = k.sb("rmlt", [128, 128], F32)
            k.ts("dve", Dpos[:], Dm[:], 0.0, None, ALU.max, R=[Dm], W=[Dpos])
            k.ts("dve", Dneg[:], Dm[:], -1.0, 0.0, ALU.mult, ALU.max, R=[Dm], W=[Dneg])
            k.ts("dve", mge[:], Dm[:], 0.0, None, ALU.is_ge, R=[Dm], W=[mge])
            k.ts("dve", mlt[:], Dm[:], 0.0, None, ALU.is_lt, R=[Dm], W=[mlt])
            MTb = k.sb("rMT", [128, 4, 128], BF16)
            ef = k.sb("ref", [128, 128], F32); ebk = k.sb("reb", [128, 128], F32)
            for h in range(4):
                k.act(ef[:], Dpos[:], AF.Exp, R=[Dpos, lg], W=[ef], scale=lg[:, h:h + 1])
                k.act(ebk[:], Dneg[:], AF.Exp, R=[Dneg, lg], W=[ebk], scale=lg[:, 4 + h:5 + h])
                k.tt("dve", ef[:], ef[:], mge[:], ALU.mult, R=[ef, mge], W=[ef])
                k.tt("dve", ebk[:], ebk[:], mlt[:], ALU.mult, R=[ebk, mlt], W=[ebk])
                k.tt("dve", ef[:], ef[:], ebk[:], ALU.add, R=[ef, ebk], W=[ef])
                k.ts("dve", MTb[:, h, :], ef[:], 0.125, None, ALU.mult, R=[ef], WP=[MTb])
            pk = k.sb("rpk", [128, 2], F32)
            k.op("pool", lambda: nc.gpsimd.iota(pk[:, 0:1], pattern=[[0, 1]], base=127, channel_multiplier=-1,
                                                allow_small_or_imprecise_dtypes=True), WP=[pk])
            k.op("pool", lambda: nc.gpsimd.iota(pk[:, 1:2], pattern=[[0, 1]], base=0, channel_multiplier=1,
                                                allow_small_or_imprecise_dtypes=True), WP=[pk])
            w8 = k.sb("rw8", [128, 8], F32)
            for h in range(4):
                k.act(w8[:, h:h + 1], pk[:, 0:1], AF.Exp, R=[pk, lg], WP=[w8], scale=lg[:, h:h + 1])
                k.act(w8[:, 4 + h:5 + h], pk[:, 1:2], AF.Exp, R=[pk, lg], WP=[w8], scale=lg[:, 4 + h:5 + h])
            WFt = k.sb("rWF", [128, 4, 64], F32); WBt = k.sb("rWB", [128, 4, 64], F32)
            k.cp("dve", WFt[:], w8[:, 0:4].unsqueeze(2).to_broadcast([128, 4, 64]), R=[w8], W=[WFt])
            k.cp("dve", WBt[:], w8[:, 4:8].unsqueeze(2).to_broadcast([128, 4, 64]), R=[w8], W=[WBt])
            cr = k.sb("rcr", [64, 2, 128], F32)
            k.op("pool", lambda: nc.gpsimd.iota(cr[:, 0, :], pattern=[[1, 128]], base=1, channel_multiplier=0,
                                                allow_small_or_imprecise_dtypes=True), WP=[cr])
            k.op("pool", lambda: nc.gpsimd.iota(cr[:, 1, :], pattern=[[-1, 128]], base=128, channel_multiplier=0,
                                                allow_small_or_imprecise_dtypes=True), WP=[cr])
            QWF = k.sb("rQWF", [64, 4, 128], F32); QWB = k.sb("rQWB", [64, 4, 128], F32)
            for h in range(4):
                k.act(QWF[:, h, :], cr[:, 0, :], AF.Exp, R=[cr, lg], WP=[QWF], scale=lg[:64, h:h + 1])
                k.act(QWB[:, h, :], cr[:, 1, :], AF.Exp, R=[cr, lg], WP=[QWB], scale=lg[:64, 4 + h:5 + h])
            k.ts("dve", QWF[:], QWF[:], 0.125, None, ALU.mult, R=[QWF], W=[QWF])
            k.ts("dve", QWB[:], QWB[:], 0.125, None, ALU.mult, R=[QWB], W=[QWB])
            g8 = k.sb("rg8", [64, 8], F32)
            k.act(g8[:], lg[:64, :], AF.Exp, R=[lg], W=[g8], scale=128.0)
            GC = k.sb("rGC", [64, 2, 4, 64], F32)
            for d_ in range(2):
                k.cp("dve", GC[:, d_], g8[:, d_ * 4:(d_ + 1) * 4].unsqueeze(2).to_broadcast([64, 4, 64]), R=[g8], WP=[GC])
            SBs = k.sb("rSB", [64, NCH, 4, 64], BF16)
            cur = [k.sb(f"rcur{i}", [64, 4, 64], F32) for i in range(2)]
            for c_ in cur:
                k.memset("dve", c_[:], 0.0, W=[c_])
            KTc = [k.sb(f"rKT{i}", [64, 4, 128], BF16) for i in range(2)]
            QTc = [k.sb(f"rQT{i}", [64, 4, 128], BF16) for i in range(2)]
            Vc = [k.sb(f"rV{i}", [128, 256], BF16) for i in range(2)]
            Gc = [k.sb(f"rG{i}", [128, 256], BF16) for i in range(2)]
            Kw = [k.sb(f"rKw{i}", [128, 256], BF16) for i in range(2)]
            SM = [k.sb(f"rSM{i}", [128, 4, 128], BF16) for i in range(2)]
            Qf = [k.sb(f"rQf{i}", [64, 4, 128], BF16) for i in range(2)]
            Qb = [k.sb(f"rQb{i}", [64, 4, 128], BF16) for i in range(2)]
            Sfb = [k.sb(f"rSf{i}", [64, 4, 64], BF16) for i in range(2)]
            sm4 = k.sb("rsm4", [128, 4], F32); vs4 = k.sb("rvs4", [128, 4], F32)
            cen = k.sb("rcen", [128, 4, 64], F32); sq = k.sb("rsq", [128, 4, 64], F32)
            res = [k.sb(f"rres{i}", [128, 256], BF16) for i in range(2)]
            rT = [k.sb(f"rrT{i}", [128, 2, 128], BF16) for i in range(2)]
            psK = k.ps("rpsK", [128, 1024], BF16)
            pkv = [k.ps(f"rpkv{i}", [128, 512]) for i in range(2)]
            pSs = [k.ps(f"rpS{i}", [128, 512]) for i in range(2)]
            pO = [k.ps(f"rpO{i}", [128, 512]) for i in range(2)]
            pT = k.ps("rpT", [128, 1024], BF16)
            self.chk("ret")
            n = 0

            def load_kv(c, i):
                cols = slice(c * 128, (c + 1) * 128)
                k.dma(KTc[i][:], self.RKT[:, cols].rearrange("(h d) t -> d h t", d=64), W=[KTc[i]])
                k.dma(Vc[i][:], self.RV[cols, :], W=[Vc[i]])

            def kv_state(i, Wt, pk_):
                for h in range(4):
                    k.tr(psK[:, h * 64:(h + 1) * 64], KTc[i][:, h, :], self.identb[:64, :64],
                         R=[KTc[i], self.identb], WP=[psK])
                k.tt("dve", Kw[i][:], psK[:, 0:256], Wt[:].rearrange("p h d -> p (h d)"), ALU.mult,
                     R=[psK, Wt], W=[Kw[i]])
                for h in range(4):
                    k.mm(pk_[:64, h * 64:(h + 1) * 64], Kw[i][:, h * 64:(h + 1) * 64], Vc[i][:, h * 64:(h + 1) * 64],
                         True, True, R=[Kw[i], Vc[i]], WP=[pk_])

            def scan(cu, d_, pk_):
                k.tt("dve", cu[:], cu[:], GC[:, d_], ALU.mult, R=[cu, GC], W=[cu])
                k.tt("dve", cu[:], cu[:], pk_[:64, 0:256].rearrange("p (h d) -> p h d", d=64), ALU.add,
                     R=[cu, pk_], W=[cu])

            def b_front(c, i):
                load_kv(c, i)
                kv_state(i, WBt, pkv[i])

            def b_back(c, i):
                k.cp("pool", SBs[:, c], cur[1][:], R=[cur[1]], WP=[SBs])
                scan(cur[1], 1, pkv[i])

            def f_front(c, i):
                cols = slice(c * 128, (c + 1) * 128)
                load_kv(c, i)
                kv_state(i, WFt, pkv[i])
                if c < NL or do_ctx:
                    pS = pSs[i]
                    k.dma(QTc[i][:], self.RQT[:, cols].rearrange("(h d) t -> d h t", d=64), W=[QTc[i]])
                    k.dma(Gc[i][:], self.RG[cols, :], W=[Gc[i]])
                    for h in range(4):
                        k.mm(pS[:, h * 128:(h + 1) * 128], KTc[i][:, h, :], QTc[i][:, h, :], True, True,
                             R=[KTc[i], QTc[i]], WP=[pS])

            def f_back(c, i):
                cols = slice(c * 128, (c + 1) * 128)
                if c < NL or do_ctx:
                    pS = pSs[i]
                    k.tt("dve", SM[i][:].rearrange("p h c -> p (h c)"), pS[:, :], MTb[:].rearrange("p h c -> p (h c)"),
                         ALU.mult, R=[pS, MTb], W=[SM[i]])
                    k.tt("pool", Qf[i][:], QTc[i][:], QWF[:], ALU.mult, R=[QTc[i], QWF], W=[Qf[i]])
                    k.tt("pool", Qb[i][:], QTc[i][:], QWB[:], ALU.mult, R=[QTc[i], QWB], W=[Qb[i]])
                    k.cp("pool", Sfb[i][:], cur[0][:], R=[cur[0]], W=[Sfb[i]])
                    po = pO[i]
                    for h in range(4):
                        osl = po[:, h * 64:(h + 1) * 64]
                        k.mm(osl, SM[i][:, h, :], Vc[i][:, h * 64:(h + 1) * 64], True, False, R=[SM[i], Vc[i]], WP=[po])
                        k.mm(osl, Qf[i][:, h, :], Sfb[i][:, h, :], False, False, R=[Qf[i], Sfb[i]], WP=[po])
                        k.mm(osl, Qb[i][:, h, :], SBs[:, c, h, :], False, True, R=[Qb[i], SBs], WP=[po])
                    po3 = po[:, 0:256].rearrange("p (h d) -> p h d", d=64)
                    k.op("dve", lambda: nc.vector.tensor_reduce(out=sm4[:], in_=po3, axis=AX.X, op=ALU.add),
                         R=[po], W=[sm4])
                    k.op("dve", lambda: nc.vector.tensor_scalar(out=sm4[:], in0=sm4[:], scalar1=-1.0 / 64, scalar2=None,
                                                                op0=ALU.mult), R=[sm4], W=[sm4])
                    k.tt("dve", cen[:], po3, sm4[:].unsqueeze(2).to_broadcast([128, 4, 64]), ALU.add,
                         R=[po, sm4], W=[cen])
                    k.tt("dve", sq[:], cen[:], cen[:], ALU.mult, R=[cen], W=[sq])
                    k.op("dve", lambda: nc.vector.tensor_reduce(out=vs4[:], in_=sq[:], axis=AX.X, op=ALU.add),
                         R=[sq], W=[vs4])
                    k.act(vs4[:], vs4[:], AF.Sqrt, R=[vs4], W=[vs4], scale=1.0 / 64, bias=self.epsb[:, 0:1])
                    k.op("dve", lambda: nc.vector.reciprocal(out=vs4[:], in_=vs4[:]), R=[vs4], W=[vs4])
                    k.tt("dve", cen[:], cen[:], vs4[:].unsqueeze(2).to_broadcast([128, 4, 64]), ALU.mult,
                         R=[cen, vs4], W=[cen])
                    k.tt("pool", res[i][:], cen[:].rearrange("p h d -> p (h d)"), Gc[i][:], ALU.mult,
                         R=[cen, Gc[i]], W=[res[i]])
                    for hp in range(2):
                        k.tr(pT[:, hp * 128:(hp + 1) * 128], res[i][:, hp * 128:(hp + 1) * 128], self.identb[:],
                             R=[res[i], self.identb], WP=[pT])
                    k.cp("act", rT[i][:].rearrange("p a t -> p (a t)"), pT[:, 0:256], R=[pT], W=[rT[i]])
                    k.dma(self.MIXT[0:256, cols].rearrange("(a d) t -> d a t", d=128), rT[i][:], R=[rT[i]], WP=[self.MIXT])
                scan(cur[0], 0, pkv[i])

            for (order, fr, bk) in ((order_b, b_front, b_back), (order_f, f_front, f_back)):
                for n_ in range(len(order) + 1):
                    if n_ < len(order):
                        fr(order[n_], n_ % 2)
                    if n_ >= 1:
                        bk(order[n_ - 1], (n_ - 1) % 2)
                k.barrier()

    def _finalize(self, po, QW, osb, pd, rec, ob, nrows=65):
        k, nc = self.k, self.nc
        k.cp("dve", osb[:65, :QW], po[:65, :QW], R=[po], W=[osb])
        k.mm(pd[:64, :QW], self.sel[:, :], osb[:65, :QW], True, True, R=[self.sel, osb], W=[pd])
        k.op("dve", lambda: nc.vector.reciprocal(out=rec[:, :QW], in_=pd[:64, :QW]), R=[pd], W=[rec])
        k.tt("pool", ob[:, :QW], osb[0:64, :QW], rec[:, :QW], ALU.mult, R=[osb, rec], W=[ob])

    def mla_phase(self, l, do_ctx):
        k, nc = self.k, self.nc
        L, LT = self.L, self.LT
        NK = LT // 128
        with Phase(k):
            KT = k.sb("mKT", [96, 4, LT], BF16)
            VA = k.sb("mVA", [128, NK, 4, 66], BF16)
            k.memset("pool", VA[:], 1.0, W=[VA])
            for h in range(4):
                for c0 in range(0, LT, 2048):
                    c1 = min(LT, c0 + 2048)
                    k.dma(KT[:, h, c0:c1], self.MKT[h * 96:(h + 1) * 96, c0:c1], WP=[KT])
            for t0 in range(NK):
                k.dma(VA[:, t0, :, 0:64],
                      self.MV[t0 * 128:(t0 + 1) * 128, :].rearrange("p (h d) -> p h d", d=64), WP=[VA])
            QT = [k.sb(f"mQT{i}", [96, 4, 512], BF16) for i in range(2)]
            eb = [k.sb(f"meb{i}", [128, 512], BF16) for i in range(4)]
            osb = [k.sb(f"mos{i}", [65, 512], F32) for i in range(2)]
            rec = [k.sb(f"mrec{i}", [64, 512], F32) for i in range(2)]
            ob = [k.sb(f"mob{i}", [64, 512], BF16) for i in range(2)]
            PS = [k.ps(f"mps{i}", [128, 512]) for i in range(4)]
            PO = [k.ps(f"mpo{i}", [128, 512]) for i in range(2)]
            PD = k.ps("mpd", [128, 512])
            self.chk("mla")
            st = {"n": 0, "q": 0, "f": 0}
            scale = float(96 ** -0.5)

            steps = []

            def qtile(q0, QW, keytiles):
                qi = st["q"]; st["q"] += 1
                for h in range(4):
                    f = st["f"]; st["f"] += 1
                    for i, kt in enumerate(keytiles):
                        steps.append((qi, q0, QW, h, f, i, kt, len(keytiles)))

            if do_ctx:
                qtile(L, LC, [L // 128, L // 128 + 1])
            for t in range(L // 512):
                qtile(t * 512, 512, list(range(NK)))

            def front(n):
                qi, q0, QW, h, f, i, kt, nk = steps[n]
                qt = QT[qi % 2]
                if h == 0 and i == 0:
                    k.dma(qt[:, :, :QW], self.MQT[:, q0:q0 + QW].rearrange("(h d) t -> d h t", d=96), W=[qt])
                ps_ = PS[n % 4]
                k.mm(ps_[:, :QW], KT[:, h, kt * 128:(kt + 1) * 128], qt[:, h, :QW], True, True, R=[KT, qt], W=[ps_])

            def back(n):
                qi, q0, QW, h, f, i, kt, nk = steps[n]
                ps_ = PS[n % 4]; e = eb[n % 4]; po = PO[f % 2]
                k.act(e[:, :QW], ps_[:, :QW], AF.Exp, R=[ps_], W=[e], scale=scale)
                k.mm(po[:65, :QW], VA[:, kt, h, 0:65], e[:, :QW], i == 0, i == nk - 1, R=[VA, e], WP=[po])
                if i == nk - 1:
                    self._finalize(po, QW, osb[f % 2], PD, rec[f % 2], ob[f % 2])
                    k.dma(self.MIXT[768 + h * 64:768 + (h + 1) * 64, q0:q0 + QW], ob[f % 2][:, :QW],
                          R=[ob[f % 2]], WP=[self.MIXT])

            LA = 3
            for n in range(len(steps) + LA):
                if n < len(steps):
                    front(n)
                if n - LA >= 0:
                    back(n - LA)

    def na_phase(self, l, do_ctx):
        k, nc = self.k, self.nc
        L, LT = self.L, self.LT
        NTQ = L // 128
        with Phase(k):
            with Phase(k):
                raw = [k.sb(f"nraw{i}", [128, 5120], F32) for i in range(2)]
                ebf = [k.sb(f"nebf{i}", [128, 5120], BF16) for i in range(2)]
                for v in range(5):
                    k.dma(raw[v % 2][:], self.natab[l, v], W=[raw[v % 2]])
                    k.act(ebf[v % 2][:], raw[v % 2][:], AF.Exp, R=[raw[v % 2]], W=[ebf[v % 2]])
                    k.dma(self.EXPB[v], ebf[v % 2][:], R=[ebf[v % 2]], WP=[self.EXPB])
            KC = k.sb("nKC", [64, 8, 256], BF16)
            VC = k.sb("nVC", [128, 2, 8, 66], BF16)
            k.memset("pool", VC[:], 1.0, W=[VC])
            k.dma(KC[:], self.NKT[:, L:L + LC].rearrange("(h d) t -> d h t", d=64), W=[KC])
            for t0 in range(2):
                k.dma(VC[:, t0, :, 0:64], self.NV[L + t0 * 128:L + (t0 + 1) * 128, :].rearrange("p (h d) -> p h d", d=64),
                      WP=[VC])
            EB = k.sb("nEB", [128, 8, 5, 128], BF16)
            QT = [k.sb(f"nQT{i}", [64, 8, 128], BF16) for i in range(2)]
            KW = [k.sb(f"nKW{i}", [64, 8, 640], BF16) for i in range(2)]
            VW = [k.sb(f"nVW{i}", [128, 5, 8, 66], BF16) for i in range(2)]
            for b_ in VW:
                k.memset("pool", b_[:], 1.0, W=[b_])
            ea = [k.sb(f"nea{i}", [128, 512], BF16) for i in range(3)]
            eb = [k.sb(f"neb{i}", [128, 384], BF16) for i in range(3)]
            pa_ = [k.sb(f"npa{i}", [128, 512], BF16) for i in range(3)]
            pb_ = [k.sb(f"npb{i}", [128, 128], BF16) for i in range(3)]
            osb = [k.sb(f"nos{i}", [65, 512], F32) for i in range(2)]
            rec = [k.sb(f"nrec{i}", [64, 512], F32) for i in range(2)]
            ob = [k.sb(f"nob{i}", [64, 512], BF16) for i in range(2)]
            PA = [k.ps(f"npsa{i}", [128, 512]) for i in range(3)]
            PBk = [k.ps(f"npsb{i}", [128, 512]) for i in range(3)]
            PO = [k.ps("npo0", [128, 512])] * 2
            PD = k.ps("npd", [128, 512])
            self.chk("na")
            st = {"n": 0, "f": 0, "var": -1}
            scale = 0.125

            def ctx_part(pb, qt, h, off):
                for c in range(2):
                    k.mm(pb[:, off + c * 128:off + (c + 1) * 128], KC[:, h, c * 128:(c + 1) * 128], qt[:, h, :],
                         True, True, R=[KC, qt], WP=[pb])

            steps = [(i, hg, hh) for i in range(NTQ) for hg in range(2) for hh in range(4)]

            def front(n):
                i, hg, hh = steps[n]
                h = hg * 4 + hh
                qt, kw, vw = QT[i % 2], KW[i % 2], VW[i % 2]

                def loads(i_):
                    lo = min(max(i_ - 2, 0), NTQ - 5)
                    qt_, kw_, vw_ = QT[i_ % 2], KW[i_ % 2], VW[i_ % 2]
                    k.dma(qt_[:], self.NQT[:, i_ * 128:(i_ + 1) * 128].rearrange("(h d) t -> d h t", d=64), W=[qt_])
                    k.dma(kw_[:], self.NKT[:, lo * 128:(lo + 5) * 128].rearrange("(h d) t -> d h t", d=64), W=[kw_])
                    for j in range(5):
                        k.dma(vw_[:, j, :, 0:64],
                              self.NV[(lo + j) * 128:(lo + j + 1) * 128, :].rearrange("p (h d) -> p h d", d=64), WP=[vw_])

                if n == 0:
                    loads(0)
                if hg == 0 and hh == 2 and i + 1 < NTQ:
                    loads(i + 1)
                pa, pb = PA[n % 3], PBk[n % 3]
                for j in range(4):
                    k.mm(pa[:, j * 128:(j + 1) * 128], kw[:, h, j * 128:(j + 1) * 128], qt[:, h, :], True, True,
                         R=[kw, qt], WP=[pa])
                k.mm(pb[:, 0:128], kw[:, h, 512:640], qt[:, h, :], True, True, R=[kw, qt], WP=[pb])
                ctx_part(pb, qt, h, 128)

            def back(n):
                i, hg, hh = steps[n]
                h = hg * 4 + hh
                vw = VW[i % 2]
                if hg == 0 and hh == 0:
                    v = na_variant(i, NTQ)
                    if v != st["var"]:
                        k.dma(EB[:], self.EXPB[v].rearrange("p (h j q) -> p h j q", h=8, j=5), W=[EB])
                        st["var"] = v
                if hh == 0:
                    st["fcur"] = st["f"]; st["f"] += 1
                f = st["fcur"]
                po = PO[f % 2]
                pa, pb = PA[n % 3], PBk[n % 3]
                e1, e2, p1, p2 = ea[n % 3], eb[n % 3], pa_[n % 3], pb_[n % 3]
                k.act(e1[:], pa[:], AF.Exp, R=[pa], W=[e1], scale=scale)
                k.act(e2[:], pb[:, 0:384], AF.Exp, R=[pb], W=[e2], scale=scale)
                k.tt("dve", p1[:], e1[:], EB[:, h, 0:4, :].rearrange("p j q -> p (j q)"), ALU.mult, R=[e1, EB], W=[p1])
                k.tt("dve", p2[:], e2[:, 0:128], EB[:, h, 4, :], ALU.mult, R=[e2, EB], W=[p2])
                osl = po[:65, hh * 128:(hh + 1) * 128]
                for j in range(4):
                    k.mm(osl, vw[:, j, h, 0:65], p1[:, j * 128:(j + 1) * 128], j == 0, False, R=[vw, p1], WP=[po])
                k.mm(osl, vw[:, 4, h, 0:65], p2[:], False, False, R=[vw, p2], WP=[po])
                for c in range(2):
                    k.mm(osl, VC[:, c, h, 0:65], e2[:, 128 + c * 128:256 + c * 128], False, c == 1, R=[VC, e2], WP=[po])
                if hh == 3:
                    self._finalize(po, 512, osb[f % 2], PD, rec[f % 2], ob[f % 2])
                    k.dma(self.MIXT[256 + hg * 256:512 + hg * 256, i * 128:(i + 1) * 128].rearrange("(h d) t -> d h t", d=64),
                          ob[f % 2][:, :].rearrange("d (h t) -> d h t", h=4), R=[ob[f % 2]], WP=[self.MIXT])

            LA = 2
            for n in range(len(steps) + LA):
                if n < len(steps):
                    front(n)
                if n >= LA:
                    back(n - LA)
            st["n"] = len(steps)
            if do_ctx:
                for qi in range(2):
                    qt = QT[qi % 2]
                    q0 = L + qi * 128
                    k.dma(qt[:], self.NQT[:, q0:q0 + 128].rearrange("(h d) t -> d h t", d=64), W=[qt])
                    for hg in range(2):
                        f = st["f"]; st["f"] += 1
                        po = PO[f % 2]
                        for hh in range(4):
                            h = hg * 4 + hh
                            n = st["n"]; st["n"] += 1
                            pb = PBk[n % 2]; e2 = eb[n % 2]
                            ctx_part(pb, qt, h, 0)
                            k.act(e2[:, 0:256], pb[:, 0:256], AF.Exp, R=[pb], W=[e2], scale=scale)
                            osl = po[:65, hh * 128:(hh + 1) * 128]
                            for c in range(2):
                                k.mm(osl, VC[:, c, h, 0:65], e2[:, c * 128:(c + 1) * 128], c == 0, c == 1,
                                     R=[VC, e2], WP=[po])
                        self._finalize(po, 512, osb[f % 2], PD, rec[f % 2], ob[f % 2])
                        k.dma(self.MIXT[256 + hg * 256:512 + hg * 256, q0:q0 + 128].rearrange("(h d) t -> d h t", d=64),
                              ob[f % 2][:, :].rearrange("d (h t) -> d h t", h=4), R=[ob[f % 2]], WP=[self.MIXT])

    def out_phase(self, l, hin, hcin, last):
        k, nc = self.k, self.nc
        L, LT = self.L, self.LT
        hout, hcout = self.H1, self.HC1
        with Phase(k):
            WO = k.sb("oWO", [128, 8, D], BF16)
            for kc in range(8):
                self.load_cast(WO[:, kc, :], self.w_out[l, kc * 128:(kc + 1) * 128, :], D, WO)
            self._lc_all = False
            GS = 8
            u2T = k.sb("ou2T", [128, 8, GS * 128], BF16)
            acc = k.sb("oacc", [128, GS, D], F32)
            gates = k.sb("ogates", [128, GS, NE], F32)
            W1e = [k.sb(f"oW1{i}", [128, 8, FF], BF16) for i in range(2)]
            W3e = [k.sb(f"oW3{i}", [128, 8, FF], BF16) for i in range(2)]
            W2e = [k.sb(f"oW2{i}", [128, 2, D], BF16) for i in range(2)]
            hs = k.sb("ohs", [128, 4, D], F32)
            w2tmp = k.sb("ow2t", [128, 2, D], BF16)
            mixT = k.sb("omix", [128, 8, 512], BF16)
            u32 = k.sb("ou32", [128, 8, 512], F32)
            tmp = [k.sb(f"otmp{i}", [128, 512], F32) for i in range(2)]
            junk = k.sb("ojunk", [128, D], BF16)
            ss = k.sb("oss", [128, 4], F32)
            s1 = [k.sb(f"os1{i}", [128, 512], F32) for i in range(2)]
            hdn = [k.sb(f"ohdn{i}", [128, 2, 512], BF16) for i in range(2)]
            sc = k.sb("osc", [128, 4, NE], F32); sel = k.sb("osel", [128, 4, NE], F32)
            eq = k.sb("oeq", [128, 4, NE], F32); sel2 = k.sb("osel2", [128, 4, NE], F32)
            m1 = k.sb("om1", [128, 16], F32); m2 = k.sb("om2", [128, 16], F32); gsm = k.sb("ogs", [128, 16], F32)
            gmx = k.sb("ogmx", [128, 4], F32); den = k.sb("oden", [128, 4], F32)
            BK = [k.ps(f"obk{i}", [128, 512]) for i in range(8)]
            PD = [BK[0], BK[1]]; PT = BK[2]; PR = BK[3]
            P1 = [BK[4], BK[0]]; P3 = [BK[5], BK[1]]
            PY = [BK[6], BK[7], BK[2], BK[3]]
            self.chk("out")
            tiles = []
            if not last:
                tiles.append((hcin[0:LC, :], hcout[0:LC, :], L, LC, 1))
            for t in range(L // 512):
                rows = slice(t * 512, (t + 1) * 512)
                tiles.append((hin[rows, :], (self.out if last else hout)[rows, :], t * 512, 512, 0))
            groups = []
            cur, used = [], 0
            for tl in tiles:
                ns = tl[3] // 128
                if used + ns > GS:
                    groups.append(cur); cur, used = [], 0
                cur.append(tl + (used,)); used += ns
            if cur:
                groups.append(cur)
            cnt = {"pd": 0, "py": 0, "e": 0, "x": 0}

            def stage_a(src, tok0, TW, sidx, so, fold2):
                NS = TW // 128
                cols = slice(tok0, tok0 + TW)
                k.dma(mixT[:, :, :TW], self.MIXT[:, cols].rearrange("(kc k) t -> k kc t", k=128), W=[mixT])
                k.dma(hs[:, :NS, :], src.rearrange("(s p) d -> p s d", p=128), W=[hs])
                for s in range(NS):
                    for half in range(2):
                        pd = PD[cnt["pd"] % 2]; tm = tmp[cnt["pd"] % 2]; cnt["pd"] += 1
                        hc = slice(half * 512, (half + 1) * 512)
                        for kc in range(8):
                            k.mm(pd[:, :], mixT[:, kc, s * 128:(s + 1) * 128], WO[:, kc, hc], kc == 0, kc == 7,
                                 R=[mixT, WO], WP=[pd])
                        if sidx == 0:
                            k.tt("dve", hs[:, s, hc], pd[:, :], hs[:, s, hc], ALU.add, R=[pd, hs], WP=[hs])
                        else:
                            k.tt("dve", tm[:], pd[:, :], self.G1B[:, sidx, hc], ALU.mult, R=[pd, self.G1B], W=[tm])
                            k.tt("pool", hs[:, s, hc], hs[:, s, hc], tm[:], ALU.add, R=[hs, tm], WP=[hs])
                if fold2:
                    k.cp("pool", acc[:, so:so + NS, :], hs[:, :NS, :], R=[hs], WP=[acc])
                else:
                    k.dma(self.HM[cols, :].rearrange("(s p) d -> p s d", p=128), hs[:, :NS, :], R=[hs], WP=[self.HM])
                for s in range(NS):
                    k.act(junk[:], hs[:, s, :], AF.Square, R=[hs], W=[junk], WP=[ss], accum_out=ss[:, s:s + 1])
                k.act(ss[:, :NS], ss[:, :NS], AF.Sqrt, R=[ss], W=[ss], scale=1.0 / D, bias=self.epsb[:, 0:1])
                k.op("dve", lambda: nc.vector.reciprocal(out=ss[:, :NS], in_=ss[:, :NS]), R=[ss], W=[ss])
                for s in range(NS):
                    k.act(hs[:, s, :], hs[:, s, :], AF.Copy, R=[hs, ss], W=[hs], scale=ss[:, s:s + 1])
                gc0 = so * 128
                for j in range(8):
                    for s in range(NS):
                        k.tr(PT[:, s * 128:(s + 1) * 128], hs[:, s, j * 128:(j + 1) * 128], self.identf[:],
                             R=[hs, self.identf], WP=[PT])
                    k.act(u32[:, j, :TW], PT[:, :TW], AF.Identity, R=[PT, self.A2, self.B2], WP=[u32],
                          scale=self.A2[:, j, sidx:sidx + 1], bias=self.B2[:, j, sidx:sidx + 1])
                    k.cp("dve", u2T[:, j, gc0:gc0 + TW], u32[:, j, :TW], R=[u32], WP=[u2T])
                for s in range(NS):
                    for kc in range(8):
                        k.mm(PR[:, s * 16:(s + 1) * 16], u32[:, kc, s * 128:(s + 1) * 128], self.rwf[:, kc, :],
                             kc == 0, kc == 7, R=[u32, self.rwf], WP=[PR])
                V4 = lambda t_: t_[:, :NS, :].rearrange("p s (g e) -> p (s g) e", e=4)
                k.act(sc[:, :NS, :], PR[:, 0:NS * 16].rearrange("p (s e) -> p s e", e=16), AF.Sigmoid, R=[PR], W=[sc])
                k.tt("dve", sel[:, :NS, :], sc[:, :NS, :], self.rbb[:].unsqueeze(1).to_broadcast([128, NS, NE]),
                     ALU.add, R=[sc, self.rbb], W=[sel])
                G4 = NS * 4
                k.op("dve", lambda: nc.vector.tensor_reduce(out=m1[:, :G4], in_=V4(sel), axis=AX.X, op=ALU.max),
                     R=[sel], W=[m1])
                k.tt("dve", V4(eq), V4(sel), m1[:, :G4].unsqueeze(2).to_broadcast([128, G4, 4]), ALU.is_equal,
                     R=[sel, m1], W=[eq])
                k.stt("dve", sel2[:, :NS, :], eq[:, :NS, :], -1.0e9, sel[:, :NS, :], ALU.mult, ALU.add,
                      R=[eq, sel], W=[sel2])
                k.op("dve", lambda: nc.vector.tensor_reduce(out=m2[:, :G4], in_=V4(sel2), axis=AX.X, op=ALU.max),
                     R=[sel2], W=[m2])
                k.tt("dve", gsm[:, :G4], m1[:, :G4], m2[:, :G4], ALU.add, R=[m1, m2], W=[gsm])
                k.op("dve", lambda: nc.vector.tensor_reduce(out=gmx[:, :NS],
                                                            in_=gsm[:, :G4].rearrange("p (s g) -> p s g", g=4),
                                                            axis=AX.X, op=ALU.max), R=[gsm], W=[gmx])
                k.tt("dve", m1[:, :G4].rearrange("p (s g) -> p s g", g=4), gsm[:, :G4].rearrange("p (s g) -> p s g", g=4),
                     gmx[:, :NS].unsqueeze(2).to_broadcast([128, NS, 4]), ALU.is_equal, R=[gsm, gmx], W=[m1])
                k.tt("dve", V4(eq), V4(sel), m2[:, :G4].unsqueeze(2).to_broadcast([128, G4, 4]), ALU.is_ge,
                     R=[sel, m2], W=[eq])
                k.tt("dve", V4(eq), V4(eq), m1[:, :G4].unsqueeze(2).to_broadcast([128, G4, 4]), ALU.mult,
                     R=[eq, m1], W=[eq])
                k.tt("dve", sel2[:, :NS, :], sc[:, :NS, :], eq[:, :NS, :], ALU.mult, R=[sc, eq], W=[sel2])
                k.op("dve", lambda: nc.vector.tensor_reduce(out=den[:, :NS], in_=sel2[:, :NS, :], axis=AX.X, op=ALU.add),
                     R=[sel2], W=[den])
                k.op("dve", lambda: nc.vector.reciprocal(out=den[:, :NS], in_=den[:, :NS]), R=[den], W=[den])
                k.tt("dve", gates[:, so:so + NS, :], sel2[:, :NS, :], den[:, :NS].unsqueeze(2).to_broadcast([128, NS, NE]),
                     ALU.mult, R=[sel2, den], WP=[gates])

            def b_front(e, we, tok0, TW, so, x_, fc):
                gc0 = so * 128
                w1, w3 = W1e[we], W3e[we]
                fs = slice(fc * 128, (fc + 1) * 128)
                for kc in range(8):
                    k.mm(P1[fc][:, :TW], w1[:, kc, fs], u2T[:, kc, gc0:gc0 + TW], kc == 0, kc == 7,
                         R=[w1, u2T], WP=[P1[fc]])
                for kc in range(8):
                    k.mm(P3[fc][:, :TW], w3[:, kc, fs], u2T[:, kc, gc0:gc0 + TW], kc == 0, kc == 7,
                         R=[w3, u2T], WP=[P3[fc]])

            def b_back(e, we, tok0, TW, so, x_, fc):
                NS = TW // 128
                w2 = W2e[we]
                hd = hdn[x_ % 2]
                sl = s1[fc]
                k.act(sl[:, :TW], P1[fc][:, :TW], AF.Silu, R=[P1[fc]], W=[sl])
                k.tt("dve", hd[:, fc, :TW], P3[fc][:, :TW], sl[:, :TW], ALU.mult, R=[P3[fc], sl], WP=[hd])
                if fc == 0:
                    return
                for s in range(NS):
                    for half in range(2):
                        py = PY[cnt["py"] % 4]; cnt["py"] += 1
                        hc = slice(half * 512, (half + 1) * 512)
                        for f2 in range(2):
                            k.mm(py[:, :], hd[:, f2, s * 128:(s + 1) * 128], w2[:, f2, hc], f2 == 0, f2 == 1,
                                 R=[hd, w2], WP=[py])
                        g_ = gates[:, so + s, e:e + 1]
                        k.stt("dve", acc[:, so + s, hc], py[:, :], g_, acc[:, so + s, hc], ALU.mult, ALU.add,
                              R=[py, gates, acc], WP=[acc])

            def load_expert(e, gi):
                we = e % 2
                if gi > 0:
                    k.dma(W1e[we][:].rearrange("p a b -> p (a b)"), self.W1BF[e], R=[self.W1BF], W=[W1e[we]])
                    k.dma(W3e[we][:].rearrange("p a b -> p (a b)"), self.W3BF[e], R=[self.W3BF], W=[W3e[we]])
                    k.dma(W2e[we][:].rearrange("p a b -> p (a b)"), self.W2BF[e], R=[self.W2BF], W=[W2e[we]])
                    return
                for half in range(2):
                    kr = slice(half * 512, (half + 1) * 512)
                    self.load_cast(W1e[we][:, half * 4:(half + 1) * 4, :],
                                   self.w1[l, e, kr, :].rearrange("(kc k) f -> k kc f", k=128), 1024, W1e[we], inner=FF)
                    self.load_cast(W3e[we][:, half * 4:(half + 1) * 4, :],
                                   self.w3[l, e, kr, :].rearrange("(kc k) f -> k kc f", k=128), 1024, W3e[we], inner=FF)
                self.load_cast(W2e[we][:, :, :], self.w2[l, e].rearrange("(fc f) j -> f fc j", f=128), 2048,
                               W2e[we], inner=D)
                k.dma(self.W1BF[e], W1e[we][:].rearrange("p a b -> p (a b)"), R=[W1e[we]], WP=[self.W1BF])
                k.dma(self.W3BF[e], W3e[we][:].rearrange("p a b -> p (a b)"), R=[W3e[we]], WP=[self.W3BF])
                k.tt("dve", w2tmp[:], W2e[we][:], self.G2B[:, 0:1, :].to_broadcast([128, 2, D]), ALU.mult,
                     R=[W2e[we], self.G2B], W=[w2tmp])
                k.dma(self.W2BF[e], w2tmp[:].rearrange("p a b -> p (a b)"), R=[w2tmp], WP=[self.W2BF])

            def stage_c2(dst, tok0, TW, so):
                NS = TW // 128
                a_ = acc[:, so:so + NS, :]
                if last:
                    for s in range(NS):
                        k.act(junk[:], acc[:, so + s, :], AF.Square, R=[acc], W=[junk], WP=[ss], accum_out=ss[:, s:s + 1])
                    k.act(ss[:, :NS], ss[:, :NS], AF.Sqrt, R=[ss], W=[ss], scale=1.0 / D, bias=self.epsb[:, 0:1])
                    k.op("dve", lambda: nc.vector.reciprocal(out=ss[:, :NS], in_=ss[:, :NS]), R=[ss], W=[ss])
                    for s in range(NS):
                        k.stt("dve", acc[:, so + s, :], acc[:, so + s, :], ss[:, s:s + 1], self.fngb[:], ALU.mult, ALU.mult,
                              R=[acc, ss, self.fngb], WP=[acc])
                k.dma(dst.rearrange("(s p) d -> p s d", p=128), a_, R=[acc])

            def stage_c(dst, tok0, TW, sidx, so):
                NS = TW // 128
                cols = slice(tok0, tok0 + TW)
                k.dma(hs[:, :NS, :], self.HM[cols, :].rearrange("(s p) d -> p s d", p=128), W=[hs])
                for s in range(NS):
                    k.tt("dve", acc[:, so + s, :], acc[:, so + s, :], self.G2B[:, sidx, :], ALU.mult,
                         R=[acc, self.G2B], WP=[acc])
                    k.tt("pool", hs[:, s, :], hs[:, s, :], acc[:, so + s, :], ALU.add, R=[hs, acc], WP=[hs])
                if last:
                    for s in range(NS):
                        k.act(junk[:], hs[:, s, :], AF.Square, R=[hs], W=[junk], WP=[ss], accum_out=ss[:, s:s + 1])
                    k.act(ss[:, :NS], ss[:, :NS], AF.Sqrt, R=[ss], W=[ss], scale=1.0 / D, bias=self.epsb[:, 0:1])
                    k.op("dve", lambda: nc.vector.reciprocal(out=ss[:, :NS], in_=ss[:, :NS]), R=[ss], W=[ss])
                    for s in range(NS):
                        k.stt("dve", hs[:, s, :], hs[:, s, :], ss[:, s:s + 1], self.fngb[:], ALU.mult, ALU.mult,
                              R=[hs, ss, self.fngb], WP=[hs])
                k.dma(dst.rearrange("(s p) d -> p s d", p=128), hs[:, :NS, :], R=[hs])

            wo_scaled = False

            def scale_wo():
                for kc in range(8):
                    k.tt("dve" if kc % 2 else "pool", WO[:, kc, :], WO[:, kc, :], self.G1B[:, 0, :], ALU.mult,
                         R=[WO, self.G1B], WP=[WO])

            for gi, grp in enumerate(groups):
                fold2 = gi > 0
                for (src, dst, tok0, TW, sidx, so) in grp:
                    if sidx == 0 and not wo_scaled:
                        scale_wo(); wo_scaled = True
                    stage_a(src, tok0, TW, sidx, so, fold2)
                if not fold2:
                    k.memset("pool", acc[:].rearrange("p a b -> p (a b)"), 0.0, W=[acc])
                units = [(e, e % 2, tok0, TW, so, ui)
                         for ui, (e, (src, dst, tok0, TW, sidx, so)) in
                         enumerate((e, g_) for e in range(NE) for g_ in grp)]
                stp = [(u, fc) for u in units for fc in range(2)]
                load_expert(0, gi)
                load_expert(1, gi)
                for n in range(len(stp) + 1):
                    if n < len(stp):
                        b_front(*stp[n][0], stp[n][1])
                    if n >= 1:
                        u, fc = stp[n - 1]
                        b_back(*u, fc)
                        if fc == 1 and (n == len(stp) or stp[n][0][0] != u[0]) and u[0] + 2 < NE:
                            load_expert(u[0] + 2, gi)
                for (src, dst, tok0, TW, sidx, so) in grp:
                    if fold2:
                        stage_c2(dst, tok0, TW, so)
                    else:
                        stage_c(dst, tok0, TW, sidx, so)
            self._lc_all = True

    def fin_phase(self, hsrc):
        k, nc = self.k, self.nc
        with Phase(k):
            hs = [k.sb(f"fh{i}", [128, 4, D], F32) for i in range(2)]
            ho = [k.sb(f"fo{i}", [128, 4, D], F32) for i in range(2)]
            junk = k.sb("fjunk", [128, D], BF16)
            ss = [k.sb(f"fss{i}", [128, 4], F32) for i in range(2)]
            for t in range(self.L // 512):
                h, o, s_ = hs[t % 2], ho[t % 2], ss[t % 2]
                rows = slice(t * 512, (t + 1) * 512)
                k.dma(h[:], hsrc[rows, :].rearrange("(s p) d -> p s d", p=128), W=[h])
                for s in range(4):
                    k.act(junk[:], h[:, s, :], AF.Square, R=[h], W=[junk], WP=[s_], accum_out=s_[:, s:s + 1])
                k.act(s_[:], s_[:], AF.Sqrt, R=[s_], W=[s_], scale=1.0 / D, bias=self.epsb[:, 0:1])
                k.op("dve", lambda: nc.vector.reciprocal(out=s_[:], in_=s_[:]), R=[s_], W=[s_])
                for s in range(4):
                    k.stt("dve", o[:, s, :], h[:, s, :], s_[:, s:s + 1], self.fngb[:],
                          ALU.mult, ALU.mult, R=[h, s_, self.fngb], WP=[o])
                k.dma(self.out[rows, :].rearrange("(s p) d -> p s d", p=128), o[:], R=[o], WP=[self.out])


_PROG_CACHE = {}


def kernel(**inputs):
    inp = {k_: np.asarray(v) for k_, v in inputs.items()}
    B, L, _ = inp["x"].shape
    if L not in _PROG_CACHE:
        _PROG_CACHE[L] = Prog(L)
    P = _PROG_CACHE[L]
    sh, per = host_layout(inp, L)
    sh["natab"] = sh["natab"].reshape(2, 5, 128, -1)
    ncores = B
    maps = [dict(sh, **per[i]) for i in range(ncores)]
    res = run_bass_kernel_spmd(P.nc, maps, core_ids=list(range(ncores)))
    out = np.stack([np.asarray(res.results[b]["out"], dtype=np.float32) for b in range(B)], axis=0)
    return out
```

```python
import numpy as np
from contextlib import ExitStack
import concourse.bass as bass
import concourse.mybir as mybir
from concourse.bass_utils import run_bass_kernel_spmd

F32 = mybir.dt.float32
BF16 = mybir.dt.bfloat16
AF = mybir.ActivationFunctionType
ALU = mybir.AluOpType
AX = mybir.AxisListType

D = 1024
GRID_W = 64
LC = 256
EPS = 1e-6
NE = 16
FF = 256
NCOL = 3232 + 256 + 256 + 32


class Buf:
    __slots__ = ("w", "r")

    def __init__(self):
        self.w = {}
        self.r = {}


class TT:
    __slots__ = ("t", "b")

    def __init__(self, t):
        self.t = t
        self.b = Buf()

    def __getitem__(self, k):
        return self.t[k]


class Eng:
    def __init__(self, name, obj, is_dma):
        self.name = name
        self.obj = obj
        self.is_dma = is_dma
        self.known = {}
        self.sems = []
        self.n = 0


class KB:
    NSLOT = 12

    def __init__(self, nc, es):
        self.nc = nc
        self.es = es
        self.engs = {}
        self.semobj = {}
        self.semval = {}
        self.clock = {}
        for name, obj, is_dma in (("pe", nc.tensor, False), ("act", nc.scalar, False),
                                  ("dve", nc.vector, False), ("pool", nc.gpsimd, False),
                                  ("sp", nc.sync, True), ("pq", nc.gpsimd, True)):
            e = Eng(name, obj, is_dma)
            ns = self.NSLOT if is_dma else 1
            if name == "pq":
                e.exec_name = "pool"
            else:
                e.exec_name = name
            for i in range(ns):
                key = f"{name}{i}"
                s = es.enter_context(nc.semaphore(f"s_{key}"))
                self.semobj[key] = s
                self.semval[key] = 0
                e.sems.append(key)
            self.engs[name] = e
        self.engs["pq"].known = self.engs["pool"].known
        self.ninstr = 0

    def _wait(self, e, key, val):
        if e.known.get(key, 0) >= val:
            return
        e.obj.wait_ge(self.semobj[key], val)
        self.ninstr += 1
        e.known[key] = val
        ck = self.clock.get((key, val))
        if ck:
            for k2, v2 in ck.items():
                if e.known.get(k2, 0) < v2:
                    e.known[k2] = v2

    def op(self, eng, fn, R=(), W=(), WP=()):
        e = self.engs[eng]
        deps = {}
        own = e.sems if not e.is_dma else ()
        for t in R:
            for k, v in t.b.w.items():
                if deps.get(k, 0) < v:
                    deps[k] = v
        for t in W:
            for k, v in t.b.w.items():
                if k in own:
                    continue
                if deps.get(k, 0) < v:
                    deps[k] = v
            for k, v in t.b.r.items():
                if k in own:
                    continue
                if deps.get(k, 0) < v:
                    deps[k] = v
        for t in WP:
            for k, v in t.b.r.items():
                if k in own:
                    continue
                if deps.get(k, 0) < v:
                    deps[k] = v
        if eng == "pe":
            deps.pop("pe0", None)
        if e.is_dma:
            key = e.sems[e.n % self.NSLOT]
            self._wait(e, key, self.semval[key])
        else:
            key = e.sems[0]
        for k, v in deps.items():
            self._wait(e, k, v)
        ins = fn()
        inc = 16 if e.is_dma else 1
        ins.then_inc(self.semobj[key], inc)
        self.semval[key] += inc
        val = self.semval[key]
        e.n += 1
        self.ninstr += 1
        ck = dict(e.known)
        self.clock[(key, val)] = ck
        for t in R:
            t.b.r[key] = val
        for t in W:
            t.b.w = {key: val}
            t.b.r = {}
        for t in WP:
            t.b.w[key] = val
        return ins

    def barrier(self, bufs=()):
        for name in ("pe", "act", "dve", "pool", "sp"):
            e = self.engs[name]
            for key, val in self.semval.items():
                if val > 0:
                    self._wait(e, key, val)
        self.clock.clear()

    def dma(self, out, in_, R=(), W=(), WP=(), q="sp"):
        e = self.engs[q]
        return self.op(q, lambda: e.obj.dma_start(out=out, in_=in_), R, W, WP)

    def mm(self, out, lhsT, rhs, start, stop, R=(), W=(), WP=()):
        nc = self.nc
        return self.op("pe", lambda: nc.tensor.matmul(out, lhsT=lhsT, rhs=rhs, start=start, stop=stop), R, W, WP)

    def tr(self, out, in_, ident, R=(), W=(), WP=()):
        nc = self.nc
        return self.op("pe", lambda: nc.tensor.transpose(out, in_, ident), R, W, WP)

    def act(self, out, in_, func, R=(), W=(), WP=(), **kw):
        nc = self.nc
        return self.op("act", lambda: nc.scalar.activation(out=out, in_=in_, func=func, **kw), R, W, WP)

    def veng(self, eng):
        return self.nc.vector if eng == "dve" else self.nc.gpsimd

    def tt(self, eng, out, in0, in1, op, R=(), W=(), WP=()):
        v = self.veng(eng)
        return self.op(eng, lambda: v.tensor_tensor(out=out, in0=in0, in1=in1, op=op), R, W, WP)

    def ts(self, eng, out, in0, s1, s2, op0, op1=None, R=(), W=(), WP=(), **kw):
        v = self.veng(eng)
        if op1 is None:
            return self.op(eng, lambda: v.tensor_scalar(out=out, in0=in0, scalar1=s1, scalar2=None, op0=op0, **kw), R, W, WP)
        return self.op(eng, lambda: v.tensor_scalar(out=out, in0=in0, scalar1=s1, scalar2=s2, op0=op0, op1=op1, **kw), R, W, WP)

    def stt(self, eng, out, in0, scalar, in1, op0, op1, R=(), W=(), WP=()):
        v = self.veng(eng)
        return self.op(eng, lambda: v.scalar_tensor_tensor(out=out, in0=in0, scalar=scalar, in1=in1, op0=op0, op1=op1), R, W, WP)

    def cp(self, eng, out, in_, R=(), W=(), WP=()):
        if eng == "act":
            nc = self.nc
            return self.op("act", lambda: nc.scalar.copy(out=out, in_=in_), R, W, WP)
        v = self.veng(eng)
        return self.op(eng, lambda: v.tensor_copy(out=out, in_=in_), R, W, WP)

    def memset(self, eng, ap, val, W=(), WP=()):
        v = self.veng(eng)
        return self.op(eng, lambda: v.memset(ap, val), (), W, WP)

    def sb(self, name, shape, dtype):
        self.uid = getattr(self, "uid", 0) + 1
        return TT(self.es.enter_context(self.nc.sbuf_tensor(f"sb{self.uid}_{name}", shape, dtype)))

    def ps(self, name, shape, dtype=F32):
        self.uid = getattr(self, "uid", 0) + 1
        return TT(self.es.enter_context(self.nc.psum_tensor(f"ps{self.uid}_{name}", shape, dtype)))

    def dram(self, name, shape, dtype, kind="Internal"):
        return TT(self.nc.dram_tensor(name, shape, dtype, kind=kind))


class Phase:
    def __init__(self, k):
        self.k = k

    def __enter__(self):
        self.saved = self.k.es
        self.st = ExitStack()
        self.st.__enter__()
        self.k.es = self.st
        return self

    def __exit__(self, *a):
        self.k.barrier()
        self.k.es = self.saved
        return self.st.__exit__(*a)


def _partner(n_half_block):
    nb = 2 * n_half_block
    return np.array([i + n_half_block if i < n_half_block else i - n_half_block for i in range(nb)])


def _rope_tables(L, ndim):
    t = np.arange(L)
    pos = ((t // GRID_W).astype(np.float32), (t % GRID_W).astype(np.float32))
    hb = ndim // 2
    half = hb // 2
    inv = (10000.0 ** (-np.arange(half, dtype=np.float32) / half)).astype(np.float32)
    cos = np.zeros((ndim, L), np.float32)
    sin = np.zeros((ndim, L), np.float32)
    for i in range(ndim):
        blk = i // hb
        ii = i % hb
        j = ii % half
        ang = (pos[blk] * inv[j]).astype(np.float32)
        cos[i] = np.cos(ang)
        sin[i] = np.sin(ang) * (-1.0 if ii < half else 1.0)
    return cos, sin


def _perm_cols(ndim):
    hb = ndim // 2
    p = _partner(hb // 2)
    return np.concatenate([p + b * hb for b in range(2)])


def _na_tables(rpb, L):
    rows = L // GRID_W
    ntq = L // 128
    out = np.empty((5, 128, 8, 5, 128), np.float32)
    reps = [0, 1, 2, ntq - 2, ntq - 1]
    for v, i in enumerate(reps):
        lo = min(max(i - 2, 0), ntq - 5)
        q = i * 128 + np.arange(128)
        qr, qc = q // GRID_W, q % GRID_W
        r0 = np.clip(qr - 4, 0, rows - 8)
        c0 = np.clip(qc - 8, 0, GRID_W - 16)
        for j in range(5):
            kk = (lo + j) * 128 + np.arange(128)
            kr, kc = kk // GRID_W, kk % GRID_W
            valid = ((kr[:, None] >= r0[None]) & (kr[:, None] < r0[None] + 8) &
                     (kc[:, None] >= c0[None]) & (kc[:, None] < c0[None] + 16))
            dr = np.clip(kr[:, None] - qr[None] + 7, 0, 14)
            dc = np.clip(kc[:, None] - qc[None] + 15, 0, 30)
            g = rpb[:, dr, dc]
            out[v, :, :, j, :] = np.where(valid[None], g, np.float32(-100.0)).transpose(1, 0, 2)
    return out


def na_variant(i, ntq):
    if i < 2:
        return i
    if i >= ntq - 2:
        return 5 - (ntq - i)
    return 2


def host_layout(inp, L):
    f32 = np.float32
    sh = {}
    w_in = inp["w_in"]
    p64 = _perm_cols(64)
    p32 = _perm_cols(32)
    rq_rot = np.concatenate([w_in[:, :, 0 + h * 64 + p64] for h in range(4)], axis=-1)
    rk_rot = np.concatenate([w_in[:, :, 256 + h * 64 + p64] for h in range(4)], axis=-1)
    kpe_rot = w_in[:, :, 3200 + p32]
    sh["w_in"] = np.ascontiguousarray(np.concatenate([w_in, rq_rot, rk_rot, kpe_rot], axis=-1))
    w_uq = inp["w_uq"]
    nope = np.concatenate([w_uq[:, :, h * 96:h * 96 + 64] for h in range(4)], axis=-1)
    pe = np.concatenate([w_uq[:, :, h * 96 + 64:h * 96 + 96] for h in range(4)], axis=-1)
    pe_rot = np.concatenate([w_uq[:, :, h * 96 + 64 + p32] for h in range(4)], axis=-1)
    sh["w_uq"] = np.ascontiguousarray(np.concatenate([nope, pe, pe_rot], axis=-1))
    w_ukv = inp["w_ukv"]
    kn = np.concatenate([w_ukv[:, :, h * 128:h * 128 + 64] for h in range(4)], axis=-1)
    vv = np.concatenate([w_ukv[:, :, h * 128 + 64:h * 128 + 128] for h in range(4)], axis=-1)
    sh["w_ukv"] = np.ascontiguousarray(np.concatenate([kn, vv], axis=-1))
    sh["w_mod"] = inp["w_mod"]
    sh["b_mod"] = np.ascontiguousarray(inp["b_mod"].reshape(2, 48, 128).transpose(0, 2, 1))
    sh["n1g"] = np.ascontiguousarray(inp["norm1_g"].reshape(2, 8, 128).transpose(0, 2, 1))
    sh["n2g"] = np.ascontiguousarray(inp["norm2_g"].reshape(2, 8, 128).transpose(0, 2, 1))
    sh["decay"] = np.ascontiguousarray(np.concatenate([inp["ret_decay_f"], inp["ret_decay_b"]], axis=-1))
    sh["natab"] = np.stack([_na_tables(inp["na_rpb"][l], L) for l in range(2)])
    sh["qn"] = np.ascontiguousarray(inp["mla_q_norm"].reshape(2, 3, 128).transpose(0, 2, 1))
    sh["kvn"] = np.ascontiguousarray(inp["mla_kv_norm"].reshape(2, 2, 128).transpose(0, 2, 1))
    sh["w_out"] = inp["w_out"]
    sh["router_w"] = inp["router_w"]
    sh["router_b"] = np.ascontiguousarray(inp["router_b"].reshape(1, 16))
    sh["w1"] = inp["w1"]
    sh["w3"] = inp["w3"]
    sh["w2"] = inp["w2"]
    sh["fng"] = np.ascontiguousarray(inp["final_norm_g"].reshape(1, D))
    c64, s64 = _rope_tables(L, 64)
    sh["rtab"] = np.stack([np.concatenate([c64, c64]), np.concatenate([s64, s64])])
    c32, s32 = _rope_tables(L, 32)
    sh["mtab"] = np.stack([np.tile(c32, (4, 1)), np.tile(s32, (4, 1))])
    for kk in list(sh):
        sh[kk] = np.ascontiguousarray(sh[kk], dtype=f32)
    per = []
    B = inp["x"].shape[0]
    for b in range(B):
        cv = np.stack([inp["c"][b], inp["c_ctx"]], axis=-1)
        cv = cv.reshape(8, 128, 2).transpose(1, 0, 2)
        per.append({"x": np.ascontiguousarray(inp["x"][b], dtype=f32),
                    "ctx": np.ascontiguousarray(inp["ctx"][b], dtype=f32),
                    "cvec": np.ascontiguousarray(cv, dtype=f32)})
    return sh, per


class Prog:
    def __init__(self, L, depth=2, stop_after=None, dbg=(), cut=99):
        self.cut = cut
        self.L = L
        self.LT = L + LC
        self.depth = depth
        self.dbg = set(dbg)
        self.stop_after = stop_after
        nc = bass.Bass("TRN2", target_bir_lowering=False)
        self.nc = nc
        self.root = ExitStack()
        self.root.__enter__()
        self.k = KB(nc, self.root)
        self._decl()
        self._globals()
        self._run()
        self.k.barrier()
        self.root.__exit__(None, None, None)

    def _in(self, name, shape):
        return TT(self.nc.dram_tensor(name, list(shape), F32, kind="ExternalInput").ap())

    def _scr(self, name, shape, dtype):
        kind = "ExternalOutput" if name in self.dbg else "Internal"
        return TT(self.nc.dram_tensor(name, list(shape), dtype, kind=kind).ap())

    def _decl(self):
        L, LT = self.L, self.LT
        i = self._in
        self.x = i("x", (L, D)); self.ctx = i("ctx", (LC, D)); self.cvec = i("cvec", (128, 8, 2))
        self.w_mod = i("w_mod", (2, D, 6 * D)); self.b_mod = i("b_mod", (2, 128, 48))
        self.n1g = i("n1g", (2, 128, 8)); self.n2g = i("n2g", (2, 128, 8))
        self.w_in = i("w_in", (2, D, NCOL)); self.decay = i("decay", (2, 8))
        self.natab = i("natab", (2, 5, 128, 8 * 5 * 128))
        self.qn = i("qn", (2, 128, 3)); self.kvn = i("kvn", (2, 128, 2))
        self.w_uq = i("w_uq", (2, 384, 512)); self.w_ukv = i("w_ukv", (2, 256, 512))
        self.w_out = i("w_out", (2, D, D)); self.router_w = i("router_w", (D, NE))
        self.router_b = i("router_b", (1, NE))
        self.w1 = i("w1", (2, NE, D, FF)); self.w3 = i("w3", (2, NE, D, FF)); self.w2 = i("w2", (2, NE, FF, D))
        self.fng = i("fng", (1, D)); self.rtab = i("rtab", (2, 128, L)); self.mtab = i("mtab", (2, 128, L))
        self.out = TT(self.nc.dram_tensor("out", [L, D], F32, kind="ExternalOutput").ap())
        s = self._scr
        self.RQT = s("RQT", (256, LT), BF16); self.RKT = s("RKT", (256, LT), BF16)
        self.RV = s("RV", (LT, 256), BF16); self.RG = s("RG", (LT, 256), BF16)
        self.NQT = s("NQT", (512, LT), BF16); self.NKT = s("NKT", (512, LT), BF16)
        self.NV = s("NV", (LT, 512), BF16)
        self.MQT = s("MQT", (384, LT), BF16); self.MKT = s("MKT", (384, LT), BF16)
        self.MV = s("MV", (LT, 256), BF16)
        self.MIXT = s("MIXT", (D, LT), BF16)
        self.H1 = s("H1", (L, D), F32); self.HC1 = s("HC1", (LC, D), F32)
        self.HM = s("HM", (LT, D), F32)
        self.EXPB = s("EXPB", (5, 128, 8 * 5 * 128), BF16)
        self.W1BF = s("W1BF", (NE, 128, 8 * FF), BF16); self.W3BF = s("W3BF", (NE, 128, 8 * FF), BF16)
        self.W2BF = s("W2BF", (NE, 128, 2 * D), BF16)

    def _globals(self):
        k, nc = self.k, self.nc
        self.identf = k.sb("identf", [128, 128], F32)
        self.identb = k.sb("identb", [128, 128], BF16)
        self.onesb = k.sb("onesb", [128, 128], BF16)
        self.onesf = k.sb("onesf", [128, 128], F32)
        self.sel = k.sb("sel", [65, 64], F32)
        idf = self.identf
        k.memset("pool", idf[:], 1.0, W=[idf])
        k.op("pool", lambda: nc.gpsimd.affine_select(out=idf[:], in_=idf[:], pattern=[[-1, 128]],
                                                     compare_op=ALU.is_equal, fill=0.0, base=0,
                                                     channel_multiplier=1), R=[idf], W=[idf])
        k.cp("dve", self.identb[:], idf[:], R=[idf], W=[self.identb])
        k.memset("dve", self.onesb[:], 1.0, W=[self.onesb])
        k.memset("dve", self.onesf[:], 1.0, W=[self.onesf])
        k.memset("dve", self.sel[:], 0.0, W=[self.sel])
        k.memset("dve", self.sel[64:65, :], 1.0, WP=[self.sel])
        self.epsb = k.sb("epsb", [128, 1], F32)
        k.memset("dve", self.epsb[:], EPS, W=[self.epsb])
        self.MT = k.sb("MT", [128, 48, 2], F32)
        self.A1 = k.sb("A1", [128, 8, 2], F32); self.B1 = k.sb("B1", [128, 8, 2], F32)
        self.A2 = k.sb("A2", [128, 8, 2], F32); self.B2 = k.sb("B2", [128, 8, 2], F32)
        self.G1B = k.sb("G1B", [128, 2, D], F32); self.G2B = k.sb("G2B", [128, 2, D], F32)
        self.rwf = k.sb("rwf", [128, 8, NE], F32)
        self.rbb = k.sb("rbb", [128, NE], F32)
        self.fngb = k.sb("fngb", [128, D], F32)
        k.dma(self.rwf[:], self.router_w[:, :].rearrange("(kc k) e -> k kc e", k=128), W=[self.rwf])
        k.dma(self.rbb[:], self.router_b[0:1, :].partition_broadcast(128), W=[self.rbb])
        k.dma(self.fngb[:], self.fng[0:1, :].partition_broadcast(128), W=[self.fngb])

    def _run(self):
        if self.stop_after == "glob":
            return
        for l in range(self.depth):
            last = l == self.depth - 1
            hin = self.x if l == 0 else self.H1
            hcin = self.ctx if l == 0 else self.HC1
            self.mod_phase(l)
            if self.stop_after == "mod":
                return
            self.proj_phase(l, hin, hcin)
            if self.stop_after == "proj":
                return
            self.ret_phase(l, not last)
            if self.stop_after == "ret":
                return
            self.na_phase(l, not last)
            if self.stop_after == "na":
                return
            self.mla_phase(l, not last)
            if self.stop_after == "mla":
                return
            self.out_phase(l, hin, hcin, last)
            if self.stop_after == f"out{l}":
                return

    def chk(self, name):
        used = 229376 - self.nc.sbuf_bytes_remaining
        print(f"[sbuf] {name}: used {used} B/partition", flush=True)
        assert used <= 226000, (name, used)

    def load_cast(self, dst_ap, src_ap, n, dstT, rows=128, inner=None):
        k = self.k
        key = id(k.es)
        if getattr(self, "_stg_key", None) != key:
            self._stg_key = key
            self._stg = [k.sb(f"stg{i}", [128, 2048], F32) for i in range(3)]
            self._stg_n = 0
        i = self._stg_n; self._stg_n += 1
        stg = self._stg[i % 3]
        sv = stg[:rows, :n]
        if inner is not None:
            sv = sv.rearrange("p (a b) -> p a b", b=inner)
        k.dma(sv, src_ap, W=[stg])
        k.cp(("pool", "dve", "act")[i % 3] if getattr(self, "_lc_all", True) else "pool", dst_ap, sv, R=[stg], WP=[dstT])

    def mod_phase(self, l):
        k, nc = self.k, self.nc
        with Phase(k):
            ct = k.sb("ct", [128, 8, 2], F32)
            sct = k.sb("sct", [128, 8, 2], F32)
            bm = k.sb("bm", [128, 48], F32)
            g1 = k.sb("n1", [128, 8], F32); g2 = k.sb("n2", [128, 8], F32)
            k.dma(ct[:], self.cvec[:, :, :], W=[ct])
            k.dma(bm[:], self.b_mod[l], W=[bm])
            k.dma(g1[:], self.n1g[l], W=[g1])
            k.dma(g2[:], self.n2g[l], W=[g2])
            k.act(sct[:], ct[:], AF.Silu, R=[ct], W=[sct])
            psm = k.ps("psm", [128, 96])
            wm = [k.sb(f"wm{i}", [128, 8, 512], F32) for i in range(2)]
            for cg in range(12):
                w = wm[cg % 2]
                k.dma(w[:], self.w_mod[l, :, cg * 512:(cg + 1) * 512].rearrange("(kc k) j -> k kc j", k=128),
                      W=[w], q="sp")
                for jc in range(4):
                    ch = cg * 4 + jc
                    for kc in range(8):
                        k.mm(psm[:, ch * 2:ch * 2 + 2], w[:, kc, jc * 128:(jc + 1) * 128], sct[:, kc, :],
                             kc == 0, kc == 7, R=[w, sct], WP=[psm])
            MT = self.MT
            k.tt("dve", MT[:], psm[:, :].rearrange("p (c s) -> p c s", s=2),
                 bm[:].unsqueeze(2).to_broadcast([128, 48, 2]), ALU.add, R=[psm, bm], W=[MT])
            for (A, Bv, g, sc0, sh0) in ((self.A1, self.B1, g1, 8, 0), (self.A2, self.B2, g2, 32, 24)):
                k.stt("dve", A[:], MT[:, sc0:sc0 + 8, :], 1.0, g[:].unsqueeze(2).to_broadcast([128, 8, 2]),
                      ALU.add, ALU.mult, R=[MT, g], W=[A])
                k.cp("dve", Bv[:], MT[:, sh0:sh0 + 8, :], R=[MT], W=[Bv])
            xs = [k.sb(f"xb{i}", [128, 128], F32) for i in range(4)]
            pg = [k.ps(f"pg{i}", [128, 512]) for i in range(2)]
            n = 0
            for (G, c0) in ((self.G1B, 16), (self.G2B, 40)):
                for s in range(2):
                    for half in range(2):
                        p = pg[n % 2]
                        for j in range(4):
                            X = xs[(n * 4 + j) % 4]
                            k.act(X[:], self.onesf[:], AF.Copy, R=[self.onesf, MT], W=[X],
                                  scale=MT[:, c0 + half * 4 + j, s:s + 1])
                            k.mm(p[:, j * 128:(j + 1) * 128], X[:], self.identf[:], True, True,
                                 R=[X, self.identf], WP=[p])
                        k.cp("act", G[:, s, half * 512:(half + 1) * 512], p[:], R=[p], WP=[G])
                        n += 1

    def proj_phase(self, l, hin, hcin):
        k, nc = self.k, self.nc
        L, LT = self.L, self.LT
        with Phase(k):
            WI = k.sb("WI", [128, 8, NCOL], BF16)
            WUQ = k.sb("WUQ", [128, 3, 512], BF16)
            WUKV = k.sb("WUKV", [128, 2, 512], BF16)
            with Phase(k):
                for kc in range(8):
                    for (c0, c1) in ((0, 1888), (1888, NCOL)):
                        self.load_cast(WI[:, kc, c0:c1], self.w_in[l, kc * 128:(kc + 1) * 128, c0:c1], c1 - c0, WI)
                for kc in range(3):
                    self.load_cast(WUQ[:, kc, :], self.w_uq[l, kc * 128:(kc + 1) * 128, :], 512, WUQ)
                for kc in range(2):
                    self.load_cast(WUKV[:, kc, :], self.w_ukv[l, kc * 128:(kc + 1) * 128, :], 512, WUKV)
            qn = k.sb("qn", [128, 3], F32); kvn = k.sb("kvn", [128, 2], F32)
            k.dma(qn[:], self.qn[l], W=[qn]); k.dma(kvn[:], self.kvn[l], W=[kvn])
            hs = [k.sb(f"hs{i}", [128, 4, D], F32) for i in range(2)]
            junk = k.sb("junk", [128, D], BF16)
            ss = k.sb("ss", [128, 4], F32); rs = k.sb("rs", [128, 4], F32)
            xn = k.sb("xn", [128, 4, D], BF16)
            uT = k.sb("uT", [128, 8, 512], BF16)
            tabs = [k.sb(f"tab{i}", [128, 4, 512], F32) for i in range(2)]
            t1 = [k.sb(f"t1_{i}", [128, 512], F32) for i in range(2)]
            t2 = [k.sb(f"t2_{i}", [128, 512], F32) for i in range(2)]
            ob = [k.sb(f"ob{i}", [128, 512], BF16) for i in range(6)]
            sq = [k.sb(f"sq{i}", [128, 512], BF16) for i in range(3)]
            rstd = k.sb("rstd", [128, 512], F32)
            cqn = k.sb("cqn", [128, 3, 512], BF16)
            ckvn = k.sb("ckvn", [128, 2, 512], BF16)
            rvst = [k.sb(f"rvst{i}", [128, 4, 256], BF16) for i in range(1)]
            rgst = [k.sb(f"rgst{i}", [128, 4, 256], BF16) for i in range(1)]
            nvst = [k.sb(f"nvst{i}", [128, 4, 512], BF16) for i in range(1)]
            mvst = [k.sb(f"mvst{i}", [128, 4, 256], BF16) for i in range(1)]
            psT = [k.ps(f"psT{i}", [128, 1024], BF16) for i in range(2)]
            PB = [k.ps(f"pb{i}", [128, 512]) for i in range(6)]
            st = {"pb": 0, "ob": 0, "tile": 0, "ve": 0}
            self.chk("proj")

            def nps():
                p = PB[st["pb"] % 6]; st["pb"] += 1
                return p

            def nob():
                o = ob[st["ob"] % 6]; st["ob"] += 1
                return o

            def ve():
                st["ve"] += 1
                return "dve" if st["ve"] % 2 else "pool"

            def tile(src_rows, tok0, TW, sidx):
                NS = TW // 128
                ti = st["tile"]; st["tile"] += 1
                lat = sidx == 0
                cols = slice(tok0, tok0 + TW)
                h = hs[ti % 2]
                tb = tabs[ti % 2]

                def loads(j):
                    src_j, tok_j, TW_j, sidx_j = tlist[j]
                    h_j, tb_j = hs[j % 2], tabs[j % 2]
                    k.dma(h_j[:, :TW_j // 128, :], src_j.rearrange("(s p) d -> p s d", p=128), W=[h_j])
                    if sidx_j == 0:
                        k.dma(tb_j[:, 0:2, :TW_j], self.rtab[:, :, tok_j:tok_j + TW_j].rearrange("a p t -> p a t"), WP=[tb_j])
                        k.dma(tb_j[:, 2:4, :TW_j], self.mtab[:, :, tok_j:tok_j + TW_j].rearrange("a p t -> p a t"), WP=[tb_j])

                if ti == 0:
                    loads(0)
                if ti + 1 < len(tlist):
                    loads(ti + 1)
                if self.cut <= 1:
                    return
                for s in range(NS):
                    k.act(junk[:], h[:, s, :], AF.Square, R=[h], W=[junk], WP=[ss], accum_out=ss[:, s:s + 1])
                k.act(rs[:, :NS], ss[:, :NS], AF.Sqrt, R=[ss], W=[rs], scale=1.0 / D, bias=self.epsb[:, 0:1])
                k.op("dve", lambda: nc.vector.reciprocal(out=rs[:, :NS], in_=rs[:, :NS]), R=[rs], W=[rs])
                for s in range(NS):
                    k.act(xn[:, s, :], h[:, s, :], AF.Copy, R=[h, rs], WP=[xn], scale=rs[:, s:s + 1])
                if self.cut <= 2:
                    return
                for j in range(8):
                    pt = psT[j % 2]
                    for s in range(NS):
                        k.tr(pt[:, s * 128:(s + 1) * 128], xn[:, s, j * 128:(j + 1) * 128], self.identb[:],
                             R=[xn, self.identb], WP=[pt])
                    k.act(uT[:, j, :TW], pt[:, :TW], AF.Identity, R=[pt, self.A1, self.B1], WP=[uT],
                          scale=self.A1[:, j, sidx:sidx + 1], bias=self.B1[:, j, sidx:sidx + 1])

                def fm(cs, ncol, W_=WI, nk=8, rhs=uT):
                    p = nps()
                    for kc in range(nk):
                        k.mm(p[:ncol, :TW], W_[:, kc, cs:cs + ncol], rhs[:, kc, :TW], kc == 0, kc == nk - 1,
                             R=[W_, rhs], WP=[p])
                    return p

                def rope(pa, pb_, ncol, ci, si):
                    o = nob()
                    if lat:
                        a = t1[st["ob"] % 2]; b = t2[st["ob"] % 2]
                        k.tt("dve", a[:ncol, :TW], pa[:ncol, :TW], tb[:ncol, ci, :TW], ALU.mult, R=[pa, tb], W=[a])
                        k.tt("dve", b[:ncol, :TW], pb_[:ncol, :TW], tb[:ncol, si, :TW], ALU.mult, R=[pb_, tb], W=[b])
                        k.tt("pool", o[:ncol, :TW], a[:ncol, :TW], b[:ncol, :TW], ALU.add, R=[a, b], W=[o])
                    else:
                        k.cp("act", o[:ncol, :TW], pa[:ncol, :TW], R=[pa], W=[o])
                    return o

                if self.cut <= 3:
                    return
                for (base, rbase, dst) in ((0, 3232, self.RQT), (256, 3488, self.RKT)):
                    for hp in range(2):
                        pa = fm(base + hp * 128, 128)
                        pb_ = fm(rbase + hp * 128, 128) if lat else None
                        o = rope(pa, pb_, 128, 0, 1)
                        k.dma(dst[hp * 128:(hp + 1) * 128, cols], o[:, :TW], R=[o], WP=[dst])
                if self.cut <= 4:
                    return
                for (base, dst) in ((1024, self.NQT), (1536, self.NKT)):
                    for c in range(4):
                        pa = fm(base + c * 128, 128)
                        o = nob()
                        k.cp("act" if c % 2 else "dve", o[:, :TW], pa[:, :TW], R=[pa], W=[o])
                        k.dma(dst[c * 128:(c + 1) * 128, cols], o[:, :TW], R=[o], WP=[dst])
                if self.cut <= 5:
                    return
                rv_, rg_, nv_, mv_ = rvst[0], rgst[0], nvst[0], mvst[0]
                for s in range(NS):
                    import os
                    T = os.environ.get("TOG", "")
                    p = nps()
                    for kc in range(8):
                        if "g" in T:
                            break
                        k.mm(p[:, :], uT[:, kc, s * 128:(s + 1) * 128], WI[:, kc, 512:1024], kc == 0, kc == 7,
                             R=[WI, uT], WP=[p])
                    if "a" not in T:
                        k.cp("act", rv_[:, s, :], p[:, 0:256], R=[p], WP=[rv_])
                    if "b" not in T:
                        k.act(rg_[:, s, :], p[:, 256:512], AF.Silu, R=[p], WP=[rg_])
                    if "c" in T:
                        continue
                    p = nps()
                    for kc in range(8):
                        cofs = 512 if "f" in T else 2048
                        k.mm(p[:, :], uT[:, kc, s * 128:(s + 1) * 128], WI[:, kc, cofs:cofs + 512], kc == 0, kc == 7,
                             R=[WI, uT], WP=[p])
                    if "d" in T:
                        continue
                    k.cp("act" if "e" in T else "dve", nv_[:, s, :], p[:, :], R=[p], WP=[nv_])
                rows = slice(tok0, tok0 + TW)
                if self.cut <= 5.05:
                    return
                k.dma(self.RV[rows, :].rearrange("(s p) c -> p s c", p=128), rv_[:, :NS, :], R=[rv_], WP=[self.RV])
                if self.cut <= 5.1:
                    return
                k.dma(self.RG[rows, :].rearrange("(s p) c -> p s c", p=128), rg_[:, :NS, :], R=[rg_], WP=[self.RG])
                if self.cut <= 5.2:
                    return
                k.dma(self.NV[rows, :].rearrange("(s p) c -> p s c", p=128),
                      nv_[:, :NS, :], R=[nv_], WP=[self.NV])
                if self.cut <= 6:
                    return
                pcs = [fm(2560 + c * 128, 128) for c in range(3)]
                for c in range(3):
                    k.act(sq[c][:, :TW], pcs[c][:, :TW], AF.Square, R=[pcs[c]], W=[sq[c]])
                pss = nps()
                for c in range(3):
                    k.mm(pss[:, :TW], self.onesb[:], sq[c][:, :TW], c == 0, c == 2, R=[self.onesb, sq[c]], WP=[pss])
                k.act(rstd[:, :TW], pss[:, :TW], AF.Sqrt, R=[pss], W=[rstd], scale=1.0 / 384, bias=self.epsb[:, 0:1])
                k.op("dve", lambda: nc.vector.reciprocal(out=rstd[:, :TW], in_=rstd[:, :TW]), R=[rstd], W=[rstd])
                for c in range(3):
                    k.stt("dve", cqn[:, c, :TW], pcs[c][:, :TW], qn[:, c:c + 1], rstd[:, :TW], ALU.mult, ALU.mult,
                          R=[pcs[c], qn, rstd], WP=[cqn])
                for c in range(2):
                    pa = fm(c * 128, 128, WUQ, 3, cqn)
                    o = nob()
                    k.cp("act", o[:, :TW], pa[:, :TW], R=[pa], W=[o])
                    for hh in range(2):
                        hd = 2 * c + hh
                        k.dma(self.MQT[hd * 96:hd * 96 + 64, cols], o[hh * 64:(hh + 1) * 64, :TW], R=[o], WP=[self.MQT])
                pa = fm(256, 128, WUQ, 3, cqn)
                pb_ = fm(384, 128, WUQ, 3, cqn) if lat else None
                o = rope(pa, pb_, 128, 2, 3)
                for hd in range(4):
                    k.dma(self.MQT[hd * 96 + 64:hd * 96 + 96, cols], o[hd * 32:(hd + 1) * 32, :TW], R=[o], WP=[self.MQT])
                if self.cut <= 7:
                    return
                pcs = [fm(2944 + c * 128, 128) for c in range(2)]
                for c in range(2):
                    k.act(sq[c][:, :TW], pcs[c][:, :TW], AF.Square, R=[pcs[c]], W=[sq[c]])
                pss = nps()
                for c in range(2):
                    k.mm(pss[:, :TW], self.onesb[:], sq[c][:, :TW], c == 0, c == 1, R=[self.onesb, sq[c]], WP=[pss])
                k.act(rstd[:, :TW], pss[:, :TW], AF.Sqrt, R=[pss], W=[rstd], scale=1.0 / 256, bias=self.epsb[:, 0:1])
                k.op("dve", lambda: nc.vector.reciprocal(out=rstd[:, :TW], in_=rstd[:, :TW]), R=[rstd], W=[rstd])
                for c in range(2):
                    k.stt("dve", ckvn[:, c, :TW], pcs[c][:, :TW], kvn[:, c:c + 1], rstd[:, :TW], ALU.mult, ALU.mult,
                          R=[pcs[c], kvn, rstd], WP=[ckvn])
                for c in range(2):
                    pa = fm(c * 128, 128, WUKV, 2, ckvn)
                    o = nob()
                    k.cp("act", o[:, :TW], pa[:, :TW], R=[pa], W=[o])
                    for hh in range(2):
                        hd = 2 * c + hh
                        k.dma(self.MKT[hd * 96:hd * 96 + 64, cols], o[hh * 64:(hh + 1) * 64, :TW], R=[o], WP=[self.MKT])
                for s in range(NS):
                    p = nps()
                    for kc in range(2):
                        k.mm(p[:, 0:256], ckvn[:, kc, s * 128:(s + 1) * 128], WUKV[:, kc, 256:512], kc == 0, kc == 1,
                             R=[WUKV, ckvn], WP=[p])
                    k.cp("dve", mv_[:, s, :], p[:, 0:256], R=[p], WP=[mv_])
                k.dma(self.MV[rows, :].rearrange("(s p) c -> p s c", p=128),
                      mv_[:, :NS, :], R=[mv_], WP=[self.MV])
                if self.cut <= 8:
                    return
                pa = fm(3200, 32)
                pb_ = fm(3744, 32) if lat else None
                o = rope(pa, pb_, 32, 2, 3)
                for hd in range(4):
                    k.dma(self.MKT[hd * 96 + 64:hd * 96 + 96, cols], o[0:32, :TW], R=[o], WP=[self.MKT])

            tlist = [(hcin[0:LC, :], L, LC, 1)] + [(hin[t * 512:(t + 1) * 512, :], t * 512, 512, 0)
                                                    for t in range(L // 512)]
            for tl in tlist:
                tile(*tl)

    def ret_phase(self, l, do_ctx):
        k, nc = self.k, self.nc
        L, LT = self.L, self.LT
        NL = L // 128
        NCH = LT // 128
        c0x, c1x = NL, NL + 1
        order_f = [c0x, c1x] + list(range(NL))
        order_b = [c1x, c0x] + list(range(NL - 1, -1, -1))
        with Phase(k):
            dec = k.sb("rdec", [128, 8], F32)
            lg = k.sb("rlg", [128, 8], F32)
            k.dma(dec[:], self.decay[l:l + 1, :].partition_broadcast(128), W=[dec])
            k.act(lg[:], dec[:], AF.Exp, R=[dec], W=[lg], scale=-1.0)
            k.ts("dve", lg[:], lg[:], 1.0, None, ALU.add, R=[lg], W=[lg])
            k.act(lg[:], lg[:], AF.Ln, R=[lg], W=[lg])
            k.ts("dve", lg[:], lg[:], -1.0, None, ALU.mult, R=[lg], W=[lg])
            Dm = k.sb("rD", [128, 128], F32)
            k.op("pool", lambda: nc.gpsimd.iota(Dm[:], pattern=[[1, 128]], base=0, channel_multiplier=-1,
                                                allow_small_or_imprecise_dtypes=True), W=[Dm])
            Dpos = k.sb("rDp", [128, 128], F32); Dneg = k.sb("rDn", [128, 128], F32)
            mge = k.sb("rmge", [128, 128], F32); mlt = k.sb("rmlt", [128, 128], F32)
            k.ts("dve", Dpos[:], Dm[:], 0.0, None, ALU.max, R=[Dm], W=[Dpos])
            k.ts("dve", Dneg[:], Dm[:], -1.0, 0.0, ALU.mult, ALU.max, R=[Dm], W=[Dneg])
            k.ts("dve", mge[:], Dm[:], 0.0, None, ALU.is_ge, R=[Dm], W=[mge])
            k.ts("dve", mlt[:], Dm[:], 0.0, None, ALU.is_lt, R=[Dm], W=[mlt])
            MTb = k.sb("rMT", [128, 4, 128], BF16)
            ef = k.sb("ref", [128, 128], F32); ebk = k.sb("reb", [128, 128], F32)
            for h in range(4):
                k.act(ef[:], Dpos[:], AF.Exp, R=[Dpos, lg], W=[ef], scale=lg[:, h:h + 1])
                k.act(ebk[:], Dneg[:], AF.Exp, R=[Dneg, lg], W=[ebk], scale=lg[:, 4 + h:5 + h])
                k.tt("dve", ef[:], ef[:], mge[:], ALU.mult, R=[ef, mge], W=[ef])
                k.tt("dve", ebk[:], ebk[:], mlt[:], ALU.mult, R=[ebk, mlt], W=[ebk])
                k.tt("dve", ef[:], ef[:], ebk[:], ALU.add, R=[ef, ebk], W=[ef])
                k.ts("dve", MTb[:, h, :], ef[:], 0.125, None, ALU.mult, R=[ef], WP=[MTb])
            pk = k.sb("rpk", [128, 2], F32)
            k.op("pool", lambda: nc.gpsimd.iota(pk[:, 0:1], pattern=[[0, 1]], base=127, channel_multiplier=-1,
                                                allow_small_or_imprecise_dtypes=True), WP=[pk])
            k.op("pool", lambda: nc.gpsimd.iota(pk[:, 1:2], pattern=[[0, 1]], base=0, channel_multiplier=1,
                                                allow_small_or_imprecise_dtypes=True), WP=[pk])
            w8 = k.sb("rw8", [128, 8], F32)
            for h in range(4):
                k.act(w8[:, h:h + 1], pk[:, 0:1], AF.Exp, R=[pk, lg], WP=[w8], scale=lg[:, h:h + 1])
                k.act(w8[:, 4 + h:5 + h], pk[:, 1:2], AF.Exp, R=[pk, lg], WP=[w8], scale=lg[:, 4 + h:5 + h])
            WFt = k.sb("rWF", [128, 4, 64], F32); WBt = k.sb("rWB", [128, 4, 64], F32)
            k.cp("dve", WFt[:], w8[:, 0:4].unsqueeze(2).to_broadcast([128, 4, 64]), R=[w8], W=[WFt])
            k.cp("dve", WBt[:], w8[:, 4:8].unsqueeze(2).to_broadcast([128, 4, 64]), R=[w8], W=[WBt])
            cr = k.sb("rcr", [64, 2, 128], F32)
            k.op("pool", lambda: nc.gpsimd.iota(cr[:, 0, :], pattern=[[1, 128]], base=1, channel_multiplier=0,
                                                allow_small_or_imprecise_dtypes=True), WP=[cr])
            k.op("pool", lambda: nc.gpsimd.iota(cr[:, 1, :], pattern=[[-1, 128]], base=128, channel_multiplier=0,
                                                allow_small_or_imprecise_dtypes=True), WP=[cr])
            QWF = k.sb("rQWF", [64, 4, 128], F32); QWB = k.sb("rQWB", [64, 4, 128], F32)
            for h in range(4):
                k.act(QWF[:, h, :], cr[:, 0, :], AF.Exp, R=[cr, lg], WP=[QWF], scale=lg[:64, h:h + 1])
                k.act(QWB[:, h, :], cr[:, 1, :], AF.Exp, R=[cr, lg], WP=[QWB], scale=lg[:64, 4 + h:5 + h])
            k.ts("dve", QWF[:], QWF[:], 0.125, None, ALU.mult, R=[QWF], W=[QWF])
            k.ts("dve", QWB[:], QWB[:], 0.125, None, ALU.mult, R=[QWB], W=[QWB])
            g8 = k.sb("rg8", [64, 8], F32)
            k.act(g8[:], lg[:64, :], AF.Exp, R=[lg], W=[g8], scale=128.0)
            GC = k.sb("rGC", [64, 2, 4, 64], F32)
            for d_ in range(2):
                k.cp("dve", GC[:, d_], g8[:, d_ * 4:(d_ + 1) * 4].unsqueeze(2).to_broadcast([64, 4, 64]), R=[g8], WP=[GC])
            SBs = k.sb("rSB", [64, NCH, 4, 64], BF16)
            cur = [k.sb(f"rcur{i}", [64, 4, 64], F32) for i in range(2)]
            for c_ in cur:
                k.memset("dve", c_[:], 0.0, W=[c_])
            KTc = [k.sb(f"rKT{i}", [64, 4, 128], BF16) for i in range(2)]
            QTc = [k.sb(f"rQT{i}", [64, 4, 128], BF16) for i in range(2)]
            Vc = [k.sb(f"rV{i}", [128, 256], BF16) for i in range(2)]
            Gc = [k.sb(f"rG{i}", [128, 256], BF16) for i in range(2)]
            Kw = [k.sb(f"rKw{i}", [128, 256], BF16) for i in range(2)]
            SM = [k.sb(f"rSM{i}", [128, 4, 128], BF16) for i in range(2)]
            Qf = [k.sb(f"rQf{i}", [64, 4, 128], BF16) for i in range(2)]
            Qb = [k.sb(f"rQb{i}", [64, 4, 128], BF16) for i in range(2)]
            Sfb = [k.sb(f"rSf{i}", [64, 4, 64], BF16) for i in range(2)]
            sm4 = k.sb("rsm4", [128, 4], F32); vs4 = k.sb("rvs4", [128, 4], F32)
            cen = k.sb("rcen", [128, 4, 64], F32); sq = k.sb("rsq", [128, 4, 64], F32)
            res = [k.sb(f"rres{i}", [128, 256], BF16) for i in range(2)]
            rT = [k.sb(f"rrT{i}", [128, 2, 128], BF16) for i in range(2)]
            psK = k.ps("rpsK", [128, 1024], BF16)
            pkv = [k.ps(f"rpkv{i}", [128, 512]) for i in range(2)]
            pSs = [k.ps(f"rpS{i}", [128, 512]) for i in range(2)]
            pO = [k.ps(f"rpO{i}", [128, 512]) for i in range(2)]
            pT = k.ps("rpT", [128, 1024], BF16)
            self.chk("ret")
            n = 0

            def load_kv(c, i):
                cols = slice(c * 128, (c + 1) * 128)
                k.dma(KTc[i][:], self.RKT[:, cols].rearrange("(h d) t -> d h t", d=64), W=[KTc[i]])
                k.dma(Vc[i][:], self.RV[cols, :], W=[Vc[i]])

            def kv_state(i, Wt, pk_):
                for h in range(4):
                    k.tr(psK[:, h * 64:(h + 1) * 64], KTc[i][:, h, :], self.identb[:64, :64],
                         R=[KTc[i], self.identb], WP=[psK])
                k.tt("dve", Kw[i][:], psK[:, 0:256], Wt[:].rearrange("p h d -> p (h d)"), ALU.mult,
                     R=[psK, Wt], W=[Kw[i]])
                for h in range(4):
                    k.mm(pk_[:64, h * 64:(h + 1) * 64], Kw[i][:, h * 64:(h + 1) * 64], Vc[i][:, h * 64:(h + 1) * 64],
                         True, True, R=[Kw[i], Vc[i]], WP=[pk_])

            def scan(cu, d_, pk_):
                k.tt("dve", cu[:], cu[:], GC[:, d_], ALU.mult, R=[cu, GC], W=[cu])
                k.tt("dve", cu[:], cu[:], pk_[:64, 0:256].rearrange("p (h d) -> p h d", d=64), ALU.add,
                     R=[cu, pk_], W=[cu])

            def b_front(c, i):
                load_kv(c, i)
                kv_state(i, WBt, pkv[i])

            def b_back(c, i):
                k.cp("pool", SBs[:, c], cur[1][:], R=[cur[1]], WP=[SBs])
                scan(cur[1], 1, pkv[i])

            def f_front(c, i):
                cols = slice(c * 128, (c + 1) * 128)
                load_kv(c, i)
                kv_state(i, WFt, pkv[i])
                if c < NL or do_ctx:
                    pS = pSs[i]
                    k.dma(QTc[i][:], self.RQT[:, cols].rearrange("(h d) t -> d h t", d=64), W=[QTc[i]])
                    k.dma(Gc[i][:], self.RG[cols, :], W=[Gc[i]])
                    for h in range(4):
                        k.mm(pS[:, h * 128:(h + 1) * 128], KTc[i][:, h, :], QTc[i][:, h, :], True, True,
                             R=[KTc[i], QTc[i]], WP=[pS])

            def f_back(c, i):
                cols = slice(c * 128, (c + 1) * 128)
                if c < NL or do_ctx:
                    pS = pSs[i]
                    k.tt("dve", SM[i][:].rearrange("p h c -> p (h c)"), pS[:, :], MTb[:].rearrange("p h c -> p (h c)"),
                         ALU.mult, R=[pS, MTb], W=[SM[i]])
                    k.tt("pool", Qf[i][:], QTc[i][:], QWF[:], ALU.mult, R=[QTc[i], QWF], W=[Qf[i]])
                    k.tt("pool", Qb[i][:], QTc[i][:], QWB[:], ALU.mult, R=[QTc[i], QWB], W=[Qb[i]])
                    k.cp("pool", Sfb[i][:], cur[0][:], R=[cur[0]], W=[Sfb[i]])
                    po = pO[i]
                    for h in range(4):
                        osl = po[:, h * 64:(h + 1) * 64]
                        k.mm(osl, SM[i][:, h, :], Vc[i][:, h * 64:(h + 1) * 64], True, False, R=[SM[i], Vc[i]], WP=[po])
                        k.mm(osl, Qf[i][:, h, :], Sfb[i][:, h, :], False, False, R=[Qf[i], Sfb[i]], WP=[po])
                        k.mm(osl, Qb[i][:, h, :], SBs[:, c, h, :], False, True, R=[Qb[i], SBs], WP=[po])
                    po3 = po[:, 0:256].rearrange("p (h d) -> p h d", d=64)
                    k.op("dve", lambda: nc.vector.tensor_reduce(out=sm4[:], in_=po3, axis=AX.X, op=ALU.add),
                         R=[po], W=[sm4])
                    k.op("dve", lambda: nc.vector.tensor_scalar(out=sm4[:], in0=sm4[:], scalar1=-1.0 / 64, scalar2=None,
                                                                op0=ALU.mult), R=[sm4], W=[sm4])
                    k.tt("dve", cen[:], po3, sm4[:].unsqueeze(2).to_broadcast([128, 4, 64]), ALU.add,
                         R=[po, sm4], W=[cen])
                    k.tt("dve", sq[:], cen[:], cen[:], ALU.mult, R=[cen], W=[sq])
                    k.op("dve", lambda: nc.vector.tensor_reduce(out=vs4[:], in_=sq[:], axis=AX.X, op=ALU.add),
                         R=[sq], W=[vs4])
                    k.act(vs4[:], vs4[:], AF.Sqrt, R=[vs4], W=[vs4], scale=1.0 / 64, bias=self.epsb[:, 0:1])
                    k.op("dve", lambda: nc.vector.reciprocal(out=vs4[:], in_=vs4[:]), R=[vs4], W=[vs4])
                    k.tt("dve", cen[:], cen[:], vs4[:].unsqueeze(2).to_broadcast([128, 4, 64]), ALU.mult,
                         R=[cen, vs4], W=[cen])
                    k.tt("pool", res[i][:], cen[:].rearrange("p h d -> p (h d)"), Gc[i][:], ALU.mult,
                         R=[cen, Gc[i]], W=[res[i]])
                    for hp in range(2):
                        k.tr(pT[:, hp * 128:(hp + 1) * 128], res[i][:, hp * 128:(hp + 1) * 128], self.identb[:],
                             R=[res[i], self.identb], WP=[pT])
                    k.cp("act", rT[i][:].rearrange("p a t -> p (a t)"), pT[:, 0:256], R=[pT], W=[rT[i]])
                    k.dma(self.MIXT[0:256, cols].rearrange("(a d) t -> d a t", d=128), rT[i][:], R=[rT[i]], WP=[self.MIXT])
                scan(cur[0], 0, pkv[i])

            for (order, fr, bk) in ((order_b, b_front, b_back), (order_f, f_front, f_back)):
                for n_ in range(len(order) + 1):
                    if n_ < len(order):
                        fr(order[n_], n_ % 2)
                    if n_ >= 1:
                        bk(order[n_ - 1], (n_ - 1) % 2)
                k.barrier()

    def _finalize(self, po, QW, osb, pd, rec, ob, nrows=65):
        k, nc = self.k, self.nc
        k.cp("dve", osb[:65, :QW], po[:65, :QW], R=[po], W=[osb])
        k.mm(pd[:64, :QW], self.sel[:, :], osb[:65, :QW], True, True, R=[self.sel, osb], W=[pd])
        k.op("dve", lambda: nc.vector.reciprocal(out=rec[:, :QW], in_=pd[:64, :QW]), R=[pd], W=[rec])
        k.tt("pool", ob[:, :QW], osb[0:64, :QW], rec[:, :QW], ALU.mult, R=[osb, rec], W=[ob])

    def mla_phase(self, l, do_ctx):
        k, nc = self.k, self.nc
        L, LT = self.L, self.LT
        NK = LT // 128
        with Phase(k):
            KT = k.sb("mKT", [96, 4, LT], BF16)
            VA = k.sb("mVA", [128, NK, 4, 66], BF16)
            k.memset("pool", VA[:], 1.0, W=[VA])
            for h in range(4):
                for c0 in range(0, LT, 2048):
                    c1 = min(LT, c0 + 2048)
                    k.dma(KT[:, h, c0:c1], self.MKT[h * 96:(h + 1) * 96, c0:c1], WP=[KT])
            for t0 in range(NK):
                k.dma(VA[:, t0, :, 0:64],
                      self.MV[t0 * 128:(t0 + 1) * 128, :].rearrange("p (h d) -> p h d", d=64), WP=[VA])
            QT = [k.sb(f"mQT{i}", [96, 4, 512], BF16) for i in range(2)]
            eb = [k.sb(f"meb{i}", [128, 512], BF16) for i in range(4)]
            osb = [k.sb(f"mos{i}", [65, 512], F32) for i in range(2)]
            rec = [k.sb(f"mrec{i}", [64, 512], F32) for i in range(2)]
            ob = [k.sb(f"mob{i}", [64, 512], BF16) for i in range(2)]
            PS = [k.ps(f"mps{i}", [128, 512]) for i in range(4)]
            PO = [k.ps(f"mpo{i}", [128, 512]) for i in range(2)]
            PD = k.ps("mpd", [128, 512])
            self.chk("mla")
            st = {"n": 0, "q": 0, "f": 0}
            scale = float(96 ** -0.5)

            steps = []

            def qtile(q0, QW, keytiles):
                qi = st["q"]; st["q"] += 1
                for h in range(4):
                    f = st["f"]; st["f"] += 1
                    for i, kt in enumerate(keytiles):
                        steps.append((qi, q0, QW, h, f, i, kt, len(keytiles)))

            if do_ctx:
                qtile(L, LC, [L // 128, L // 128 + 1])
            for t in range(L // 512):
                qtile(t * 512, 512, list(range(NK)))

            def front(n):
                qi, q0, QW, h, f, i, kt, nk = steps[n]
                qt = QT[qi % 2]
                if h == 0 and i == 0:
                    k.dma(qt[:, :, :QW], self.MQT[:, q0:q0 + QW].rearrange("(h d) t -> d h t", d=96), W=[qt])
                ps_ = PS[n % 4]
                k.mm(ps_[:, :QW], KT[:, h, kt * 128:(kt + 1) * 128], qt[:, h, :QW], True, True, R=[KT, qt], W=[ps_])

            def back(n):
                qi, q0, QW, h, f, i, kt, nk = steps[n]
                ps_ = PS[n % 4]; e = eb[n % 4]; po = PO[f % 2]
                k.act(e[:, :QW], ps_[:, :QW], AF.Exp, R=[ps_], W=[e], scale=scale)
                k.mm(po[:65, :QW], VA[:, kt, h, 0:65], e[:, :QW], i == 0, i == nk - 1, R=[VA, e], WP=[po])
                if i == nk - 1:
                    self._finalize(po, QW, osb[f % 2], PD, rec[f % 2], ob[f % 2])
                    k.dma(self.MIXT[768 + h * 64:768 + (h + 1) * 64, q0:q0 + QW], ob[f % 2][:, :QW],
                          R=[ob[f % 2]], WP=[self.MIXT])

            LA = 3
            for n in range(len(steps) + LA):
                if n < len(steps):
                    front(n)
                if n - LA >= 0:
                    back(n - LA)

    def na_phase(self, l, do_ctx):
        k, nc = self.k, self.nc
        L, LT = self.L, self.LT
        NTQ = L // 128
        with Phase(k):
            with Phase(k):
                raw = [k.sb(f"nraw{i}", [128, 5120], F32) for i in range(2)]
                ebf = [k.sb(f"nebf{i}", [128, 5120], BF16) for i in range(2)]
                for v in range(5):
                    k.dma(raw[v % 2][:], self.natab[l, v], W=[raw[v % 2]])
                    k.act(ebf[v % 2][:], raw[v % 2][:], AF.Exp, R=[raw[v % 2]], W=[ebf[v % 2]])
                    k.dma(self.EXPB[v], ebf[v % 2][:], R=[ebf[v % 2]], WP=[self.EXPB])
            KC = k.sb("nKC", [64, 8, 256], BF16)
            VC = k.sb("nVC", [128, 2, 8, 66], BF16)
            k.memset("pool", VC[:], 1.0, W=[VC])
            k.dma(KC[:], self.NKT[:, L:L + LC].rearrange("(h d) t -> d h t", d=64), W=[KC])
            for t0 in range(2):
                k.dma(VC[:, t0, :, 0:64], self.NV[L + t0 * 128:L + (t0 + 1) * 128, :].rearrange("p (h d) -> p h d", d=64),
                      WP=[VC])
            EB = k.sb("nEB", [128, 8, 5, 128], BF16)
            QT = [k.sb(f"nQT{i}", [64, 8, 128], BF16) for i in range(2)]
            KW = [k.sb(f"nKW{i}", [64, 8, 640], BF16) for i in range(2)]
            VW = [k.sb(f"nVW{i}", [128, 5, 8, 66], BF16) for i in range(2)]
            for b_ in VW:
                k.memset("pool", b_[:], 1.0, W=[b_])
            ea = [k.sb(f"nea{i}", [128, 512], BF16) for i in range(3)]
            eb = [k.sb(f"neb{i}", [128, 384], BF16) for i in range(3)]
            pa_ = [k.sb(f"npa{i}", [128, 512], BF16) for i in range(3)]
            pb_ = [k.sb(f"npb{i}", [128, 128], BF16) for i in range(3)]
            osb = [k.sb(f"nos{i}", [65, 512], F32) for i in range(2)]
            rec = [k.sb(f"nrec{i}", [64, 512], F32) for i in range(2)]
            ob = [k.sb(f"nob{i}", [64, 512], BF16) for i in range(2)]
            PA = [k.ps(f"npsa{i}", [128, 512]) for i in range(3)]
            PBk = [k.ps(f"npsb{i}", [128, 512]) for i in range(3)]
            PO = [k.ps("npo0", [128, 512])] * 2
            PD = k.ps("npd", [128, 512])
            self.chk("na")
            st = {"n": 0, "f": 0, "var": -1}
            scale = 0.125

            def ctx_part(pb, qt, h, off):
                for c in range(2):
                    k.mm(pb[:, off + c * 128:off + (c + 1) * 128], KC[:, h, c * 128:(c + 1) * 128], qt[:, h, :],
                         True, True, R=[KC, qt], WP=[pb])

            steps = [(i, hg, hh) for i in range(NTQ) for hg in range(2) for hh in range(4)]

            def front(n):
                i, hg, hh = steps[n]
                h = hg * 4 + hh
                qt, kw, vw = QT[i % 2], KW[i % 2], VW[i % 2]

                def loads(i_):
                    lo = min(max(i_ - 2, 0), NTQ - 5)
                    qt_, kw_, vw_ = QT[i_ % 2], KW[i_ % 2], VW[i_ % 2]
                    k.dma(qt_[:], self.NQT[:, i_ * 128:(i_ + 1) * 128].rearrange("(h d) t -> d h t", d=64), W=[qt_])
                    k.dma(kw_[:], self.NKT[:, lo * 128:(lo + 5) * 128].rearrange("(h d) t -> d h t", d=64), W=[kw_])
                    for j in range(5):
                        k.dma(vw_[:, j, :, 0:64],
                              self.NV[(lo + j) * 128:(lo + j + 1) * 128, :].rearrange("p (h d) -> p h d", d=64), WP=[vw_])

                if n == 0:
                    loads(0)
                if hg == 0 and hh == 2 and i + 1 < NTQ:
                    loads(i + 1)
                pa, pb = PA[n % 3], PBk[n % 3]
                for j in range(4):
                    k.mm(pa[:, j * 128:(j + 1) * 128], kw[:, h, j * 128:(j + 1) * 128], qt[:, h, :], True, True,
                         R=[kw, qt], WP=[pa])
                k.mm(pb[:, 0:128], kw[:, h, 512:640], qt[:, h, :], True, True, R=[kw, qt], WP=[pb])
                ctx_part(pb, qt, h, 128)

            def back(n):
                i, hg, hh = steps[n]
                h = hg * 4 + hh
                vw = VW[i % 2]
                if hg == 0 and hh == 0:
                    v = na_variant(i, NTQ)
                    if v != st["var"]:
                        k.dma(EB[:], self.EXPB[v].rearrange("p (h j q) -> p h j q", h=8, j=5), W=[EB])
                        st["var"] = v
                if hh == 0:
                    st["fcur"] = st["f"]; st["f"] += 1
                f = st["fcur"]
                po = PO[f % 2]
                pa, pb = PA[n % 3], PBk[n % 3]
                e1, e2, p1, p2 = ea[n % 3], eb[n % 3], pa_[n % 3], pb_[n % 3]
                k.act(e1[:], pa[:], AF.Exp, R=[pa], W=[e1], scale=scale)
                k.act(e2[:], pb[:, 0:384], AF.Exp, R=[pb], W=[e2], scale=scale)
                k.tt("dve", p1[:], e1[:], EB[:, h, 0:4, :].rearrange("p j q -> p (j q)"), ALU.mult, R=[e1, EB], W=[p1])
                k.tt("dve", p2[:], e2[:, 0:128], EB[:, h, 4, :], ALU.mult, R=[e2, EB], W=[p2])
                osl = po[:65, hh * 128:(hh + 1) * 128]
                for j in range(4):
                    k.mm(osl, vw[:, j, h, 0:65], p1[:, j * 128:(j + 1) * 128], j == 0, False, R=[vw, p1], WP=[po])
                k.mm(osl, vw[:, 4, h, 0:65], p2[:], False, False, R=[vw, p2], WP=[po])
                for c in range(2):
                    k.mm(osl, VC[:, c, h, 0:65], e2[:, 128 + c * 128:256 + c * 128], False, c == 1, R=[VC, e2], WP=[po])
                if hh == 3:
                    self._finalize(po, 512, osb[f % 2], PD, rec[f % 2], ob[f % 2])
                    k.dma(self.MIXT[256 + hg * 256:512 + hg * 256, i * 128:(i + 1) * 128].rearrange("(h d) t -> d h t", d=64),
                          ob[f % 2][:, :].rearrange("d (h t) -> d h t", h=4), R=[ob[f % 2]], WP=[self.MIXT])

            LA = 2
            for n in range(len(steps) + LA):
                if n < len(steps):
                    front(n)
                if n >= LA:
                    back(n - LA)
            st["n"] = len(steps)
            if do_ctx:
                for qi in range(2):
                    qt = QT[qi % 2]
                    q0 = L + qi * 128
                    k.dma(qt[:], self.NQT[:, q0:q0 + 128].rearrange("(h d) t -> d h t", d=64), W=[qt])
                    for hg in range(2):
                        f = st["f"]; st["f"] += 1
                        po = PO[f % 2]
                        for hh in range(4):
                            h = hg * 4 + hh
                            n = st["n"]; st["n"] += 1
                            pb = PBk[n % 2]; e2 = eb[n % 2]
                            ctx_part(pb, qt, h, 0)
                            k.act(e2[:, 0:256], pb[:, 0:256], AF.Exp, R=[pb], W=[e2], scale=scale)
                            osl = po[:65, hh * 128:(hh + 1) * 128]
                            for c in range(2):
                                k.mm(osl, VC[:, c, h, 0:65], e2[:, c * 128:(c + 1) * 128], c == 0, c == 1,
                                     R=[VC, e2], WP=[po])
                        self._finalize(po, 512, osb[f % 2], PD, rec[f % 2], ob[f % 2])
                        k.dma(self.MIXT[256 + hg * 256:512 + hg * 256, q0:q0 + 128].rearrange("(h d) t -> d h t", d=64),
                              ob[f % 2][:, :].rearrange("d (h t) -> d h t", h=4), R=[ob[f % 2]], WP=[self.MIXT])

    def out_phase(self, l, hin, hcin, last):
        k, nc = self.k, self.nc
        L, LT = self.L, self.LT
        hout, hcout = self.H1, self.HC1
        with Phase(k):
            WO = k.sb("oWO", [128, 8, D], BF16)
            for kc in range(8):
                self.load_cast(WO[:, kc, :], self.w_out[l, kc * 128:(kc + 1) * 128, :], D, WO)
            self._lc_all = True
            GS = 8
            u2T = k.sb("ou2T", [128, 8, GS * 128], BF16)
            acc = k.sb("oacc", [128, GS, D], F32)
            gates = k.sb("ogates", [128, GS, NE], F32)
            W1e = [k.sb(f"oW1{i}", [128, 8, FF], BF16) for i in range(2)]
            W3e = [k.sb(f"oW3{i}", [128, 8, FF], BF16) for i in range(2)]
            W2e = [k.sb(f"oW2{i}", [128, 2, D], BF16) for i in range(2)]
            hs = k.sb("ohs", [128, 4, D], F32)
            w2tmp = k.sb("ow2t", [128, 2, D], BF16)
            mixT = k.sb("omix", [128, 8, 512], BF16)
            u32 = k.sb("ou32", [128, 8, 512], F32)
            tmp = [k.sb(f"otmp{i}", [128, 512], F32) for i in range(2)]
            junk = k.sb("ojunk", [128, D], BF16)
            ss = k.sb("oss", [128, 4], F32)
            s1 = [k.sb(f"os1{i}", [128, 512], F32) for i in range(2)]
            hdn = [k.sb(f"ohdn{i}", [128, 2, 512], BF16) for i in range(2)]
            sc = k.sb("osc", [128, 4, NE], F32); sel = k.sb("osel", [128, 4, NE], F32)
            eq = k.sb("oeq", [128, 4, NE], F32); sel2 = k.sb("osel2", [128, 4, NE], F32)
            m1 = k.sb("om1", [128, 16], F32); m2 = k.sb("om2", [128, 16], F32); gsm = k.sb("ogs", [128, 16], F32)
            gmx = k.sb("ogmx", [128, 4], F32); den = k.sb("oden", [128, 4], F32)
            BK = [k.ps(f"obk{i}", [128, 512]) for i in range(8)]
            PD = [BK[0], BK[1]]; PT = BK[2]; PR = BK[3]
            P1 = [BK[4], BK[0]]; P3 = [BK[5], BK[1]]
            PY = [BK[6], BK[7], BK[2], BK[3]]
            self.chk("out")
            tiles = []
            if not last:
                tiles.append((hcin[0:LC, :], hcout[0:LC, :], L, LC, 1))
            for t in range(L // 512):
                rows = slice(t * 512, (t + 1) * 512)
                tiles.append((hin[rows, :], (self.out if last else hout)[rows, :], t * 512, 512, 0))
            groups = []
            cur, used = [], 0
            for tl in tiles:
                ns = tl[3] // 128
                if used + ns > GS:
                    groups.append(cur); cur, used = [], 0
                cur.append(tl + (used,)); used += ns
            if cur:
                groups.append(cur)
            cnt = {"pd": 0, "py": 0, "e": 0, "x": 0}

            def stage_a(src, tok0, TW, sidx, so, fold2):
                NS = TW // 128
                cols = slice(tok0, tok0 + TW)
                k.dma(mixT[:, :, :TW], self.MIXT[:, cols].rearrange("(kc k) t -> k kc t", k=128), W=[mixT])
                k.dma(hs[:, :NS, :], src.rearrange("(s p) d -> p s d", p=128), W=[hs])
                for s in range(NS):
                    for half in range(2):
                        pd = PD[cnt["pd"] % 2]; tm = tmp[cnt["pd"] % 2]; cnt["pd"] += 1
                        hc = slice(half * 512, (half + 1) * 512)
                        for kc in range(8):
                            k.mm(pd[:, :], mixT[:, kc, s * 128:(s + 1) * 128], WO[:, kc, hc], kc == 0, kc == 7,
                                 R=[mixT, WO], WP=[pd])
                        if sidx == 0:
                            k.tt("dve", hs[:, s, hc], pd[:, :], hs[:, s, hc], ALU.add, R=[pd, hs], WP=[hs])
                        else:
                            k.tt("dve", tm[:], pd[:, :], self.G1B[:, sidx, hc], ALU.mult, R=[pd, self.G1B], W=[tm])
                            k.tt("pool", hs[:, s, hc], hs[:, s, hc], tm[:], ALU.add, R=[hs, tm], WP=[hs])
                if fold2:
                    k.cp("pool", acc[:, so:so + NS, :], hs[:, :NS, :], R=[hs], WP=[acc])
                else:
                    k.dma(self.HM[cols, :].rearrange("(s p) d -> p s d", p=128), hs[:, :NS, :], R=[hs], WP=[self.HM])
                for s in range(NS):
                    k.act(junk[:], hs[:, s, :], AF.Square, R=[hs], W=[junk], WP=[ss], accum_out=ss[:, s:s + 1])
                k.act(ss[:, :NS], ss[:, :NS], AF.Sqrt, R=[ss], W=[ss], scale=1.0 / D, bias=self.epsb[:, 0:1])
                k.op("dve", lambda: nc.vector.reciprocal(out=ss[:, :NS], in_=ss[:, :NS]), R=[ss], W=[ss])
                for s in range(NS):
                    k.act(hs[:, s, :], hs[:, s, :], AF.Copy, R=[hs, ss], W=[hs], scale=ss[:, s:s + 1])
                gc0 = so * 128
                for j in range(8):
                    for s in range(NS):
                        k.tr(PT[:, s * 128:(s + 1) * 128], hs[:, s, j * 128:(j + 1) * 128], self.identf[:],
                             R=[hs, self.identf], WP=[PT])
                    k.act(u32[:, j, :TW], PT[:, :TW], AF.Identity, R=[PT, self.A2, self.B2], WP=[u32],
                          scale=self.A2[:, j, sidx:sidx + 1], bias=self.B2[:, j, sidx:sidx + 1])
                    k.cp("dve", u2T[:, j, gc0:gc0 + TW], u32[:, j, :TW], R=[u32], WP=[u2T])
                for s in range(NS):
                    for kc in range(8):
                        k.mm(PR[:, s * 16:(s + 1) * 16], u32[:, kc, s * 128:(s + 1) * 128], self.rwf[:, kc, :],
                             kc == 0, kc == 7, R=[u32, self.rwf], WP=[PR])
                V4 = lambda t_: t_[:, :NS, :].rearrange("p s (g e) -> p (s g) e", e=4)
                k.act(sc[:, :NS, :], PR[:, 0:NS * 16].rearrange("p (s e) -> p s e", e=16), AF.Sigmoid, R=[PR], W=[sc])
                k.tt("dve", sel[:, :NS, :], sc[:, :NS, :], self.rbb[:].unsqueeze(1).to_broadcast([128, NS, NE]),
                     ALU.add, R=[sc, self.rbb], W=[sel])
                G4 = NS * 4
                k.op("dve", lambda: nc.vector.tensor_reduce(out=m1[:, :G4], in_=V4(sel), axis=AX.X, op=ALU.max),
                     R=[sel], W=[m1])
                k.tt("dve", V4(eq), V4(sel), m1[:, :G4].unsqueeze(2).to_broadcast([128, G4, 4]), ALU.is_equal,
                     R=[sel, m1], W=[eq])
                k.stt("dve", sel2[:, :NS, :], eq[:, :NS, :], -1.0e9, sel[:, :NS, :], ALU.mult, ALU.add,
                      R=[eq, sel], W=[sel2])
                k.op("dve", lambda: nc.vector.tensor_reduce(out=m2[:, :G4], in_=V4(sel2), axis=AX.X, op=ALU.max),
                     R=[sel2], W=[m2])
                k.tt("dve", gsm[:, :G4], m1[:, :G4], m2[:, :G4], ALU.add, R=[m1, m2], W=[gsm])
                k.op("dve", lambda: nc.vector.tensor_reduce(out=gmx[:, :NS],
                                                            in_=gsm[:, :G4].rearrange("p (s g) -> p s g", g=4),
                                                            axis=AX.X, op=ALU.max), R=[gsm], W=[gmx])
                k.tt("dve", m1[:, :G4].rearrange("p (s g) -> p s g", g=4), gsm[:, :G4].rearrange("p (s g) -> p s g", g=4),
                     gmx[:, :NS].unsqueeze(2).to_broadcast([128, NS, 4]), ALU.is_equal, R=[gsm, gmx], W=[m1])
                k.tt("dve", V4(eq), V4(sel), m2[:, :G4].unsqueeze(2).to_broadcast([128, G4, 4]), ALU.is_ge,
                     R=[sel, m2], W=[eq])
                k.tt("dve", V4(eq), V4(eq), m1[:, :G4].unsqueeze(2).to_broadcast([128, G4, 4]), ALU.mult,
                     R=[eq, m1], W=[eq])
                k.tt("dve", sel2[:, :NS, :], sc[:, :NS, :], eq[:, :NS, :], ALU.mult, R=[sc, eq], W=[sel2])
                k.op("dve", lambda: nc.vector.tensor_reduce(out=den[:, :NS], in_=sel2[:, :NS, :], axis=AX.X, op=ALU.add),
                     R=[sel2], W=[den])
                k.op("dve", lambda: nc.vector.reciprocal(out=den[:, :NS], in_=den[:, :NS]), R=[den], W=[den])
                k.tt("dve", gates[:, so:so + NS, :], sel2[:, :NS, :], den[:, :NS].unsqueeze(2).to_broadcast([128, NS, NE]),
                     ALU.mult, R=[sel2, den], WP=[gates])

            def b_front(e, we, tok0, TW, so, x_, fc):
                gc0 = so * 128
                w1, w3 = W1e[we], W3e[we]
                fs = slice(fc * 128, (fc + 1) * 128)
                for kc in range(8):
                    k.mm(P1[fc][:, :TW], w1[:, kc, fs], u2T[:, kc, gc0:gc0 + TW], kc == 0, kc == 7,
                         R=[w1, u2T], WP=[P1[fc]])
                for kc in range(8):
                    k.mm(P3[fc][:, :TW], w3[:, kc, fs], u2T[:, kc, gc0:gc0 + TW], kc == 0, kc == 7,
                         R=[w3, u2T], WP=[P3[fc]])

            def b_back(e, we, tok0, TW, so, x_, fc):
                NS = TW // 128
                w2 = W2e[we]
                hd = hdn[x_ % 2]
                sl = s1[fc]
                k.act(sl[:, :TW], P1[fc][:, :TW], AF.Silu, R=[P1[fc]], W=[sl])
                k.tt("dve", hd[:, fc, :TW], P3[fc][:, :TW], sl[:, :TW], ALU.mult, R=[P3[fc], sl], WP=[hd])
                if fc == 0:
                    return
                for s in range(NS):
                    for half in range(2):
                        py = PY[cnt["py"] % 4]; cnt["py"] += 1
                        hc = slice(half * 512, (half + 1) * 512)
                        for f2 in range(2):
                            k.mm(py[:, :], hd[:, f2, s * 128:(s + 1) * 128], w2[:, f2, hc], f2 == 0, f2 == 1,
                                 R=[hd, w2], WP=[py])
                        g_ = gates[:, so + s, e:e + 1]
                        k.stt("dve", acc[:, so + s, hc], py[:, :], g_, acc[:, so + s, hc], ALU.mult, ALU.add,
                              R=[py, gates, acc], WP=[acc])

            def load_expert(e, gi):
                we = e % 2
                if gi > 0:
                    k.dma(W1e[we][:].rearrange("p a b -> p (a b)"), self.W1BF[e], R=[self.W1BF], W=[W1e[we]])
                    k.dma(W3e[we][:].rearrange("p a b -> p (a b)"), self.W3BF[e], R=[self.W3BF], W=[W3e[we]])
                    k.dma(W2e[we][:].rearrange("p a b -> p (a b)"), self.W2BF[e], R=[self.W2BF], W=[W2e[we]])
                    return
                for half in range(2):
                    kr = slice(half * 512, (half + 1) * 512)
                    self.load_cast(W1e[we][:, half * 4:(half + 1) * 4, :],
                                   self.w1[l, e, kr, :].rearrange("(kc k) f -> k kc f", k=128), 1024, W1e[we], inner=FF)
                    self.load_cast(W3e[we][:, half * 4:(half + 1) * 4, :],
                                   self.w3[l, e, kr, :].rearrange("(kc k) f -> k kc f", k=128), 1024, W3e[we], inner=FF)
                self.load_cast(W2e[we][:, :, :], self.w2[l, e].rearrange("(fc f) j -> f fc j", f=128), 2048,
                               W2e[we], inner=D)
                k.dma(self.W1BF[e], W1e[we][:].rearrange("p a b -> p (a b)"), R=[W1e[we]], WP=[self.W1BF])
                k.dma(self.W3BF[e], W3e[we][:].rearrange("p a b -> p (a b)"), R=[W3e[we]], WP=[self.W3BF])
                k.tt("dve", w2tmp[:], W2e[we][:], self.G2B[:, 0:1, :].to_broadcast([128, 2, D]), ALU.mult,
                     R=[W2e[we], self.G2B], W=[w2tmp])
                k.dma(self.W2BF[e], w2tmp[:].rearrange("p a b -> p (a b)"), R=[w2tmp], WP=[self.W2BF])

            def stage_c2(dst, tok0, TW, so):
                NS = TW // 128
                a_ = acc[:, so:so + NS, :]
                if last:
                    for s in range(NS):
                        k.act(junk[:], acc[:, so + s, :], AF.Square, R=[acc], W=[junk], WP=[ss], accum_out=ss[:, s:s + 1])
                    k.act(ss[:, :NS], ss[:, :NS], AF.Sqrt, R=[ss], W=[ss], scale=1.0 / D, bias=self.epsb[:, 0:1])
                    k.op("dve", lambda: nc.vector.reciprocal(out=ss[:, :NS], in_=ss[:, :NS]), R=[ss], W=[ss])
                    for s in range(NS):
                        k.stt("dve", acc[:, so + s, :], acc[:, so + s, :], ss[:, s:s + 1], self.fngb[:], ALU.mult, ALU.mult,
                              R=[acc, ss, self.fngb], WP=[acc])
                k.dma(dst.rearrange("(s p) d -> p s d", p=128), a_, R=[acc])

            def stage_c(dst, tok0, TW, sidx, so):
                NS = TW // 128
                cols = slice(tok0, tok0 + TW)
                k.dma(hs[:, :NS, :], self.HM[cols, :].rearrange("(s p) d -> p s d", p=128), W=[hs])
                for s in range(NS):
                    k.tt("dve", acc[:, so + s, :], acc[:, so + s, :], self.G2B[:, sidx, :], ALU.mult,
                         R=[acc, self.G2B], WP=[acc])
                    k.tt("pool", hs[:, s, :], hs[:, s, :], acc[:, so + s, :], ALU.add, R=[hs, acc], WP=[hs])
                if last:
                    for s in range(NS):
                        k.act(junk[:], hs[:, s, :], AF.Square, R=[hs], W=[junk], WP=[ss], accum_out=ss[:, s:s + 1])
                    k.act(ss[:, :NS], ss[:, :NS], AF.Sqrt, R=[ss], W=[ss], scale=1.0 / D, bias=self.epsb[:, 0:1])
                    k.op("dve", lambda: nc.vector.reciprocal(out=ss[:, :NS], in_=ss[:, :NS]), R=[ss], W=[ss])
                    for s in range(NS):
                        k.stt("dve", hs[:, s, :], hs[:, s, :], ss[:, s:s + 1], self.fngb[:], ALU.mult, ALU.mult,
                              R=[hs, ss, self.fngb], WP=[hs])
                k.dma(dst.rearrange("(s p) d -> p s d", p=128), hs[:, :NS, :], R=[hs])

            wo_scaled = False

            def scale_wo():
                for kc in range(8):
                    k.tt("dve" if kc % 2 else "pool", WO[:, kc, :], WO[:, kc, :], self.G1B[:, 0, :], ALU.mult,
                         R=[WO, self.G1B], WP=[WO])

            for gi, grp in enumerate(groups):
                fold2 = gi > 0
                for (src, dst, tok0, TW, sidx, so) in grp:
                    if sidx == 0 and not wo_scaled:
                        scale_wo(); wo_scaled = True
                    stage_a(src, tok0, TW, sidx, so, fold2)
                if not fold2:
                    k.memset("pool", acc[:].rearrange("p a b -> p (a b)"), 0.0, W=[acc])
                units = [(e, e % 2, tok0, TW, so, ui)
                         for ui, (e, (src, dst, tok0, TW, sidx, so)) in
                         enumerate((e, g_) for e in range(NE) for g_ in grp)]
                stp = [(u, fc) for u in units for fc in range(2)]
                load_expert(0, gi)
                load_expert(1, gi)
                for n in range(len(stp) + 1):
                    if n < len(stp):
                        b_front(*stp[n][0], stp[n][1])
                    if n >= 1:
                        u, fc = stp[n - 1]
                        b_back(*u, fc)
                        if fc == 1 and (n == len(stp) or stp[n][0][0] != u[0]) and u[0] + 2 < NE:
                            load_expert(u[0] + 2, gi)
                for (src, dst, tok0, TW, sidx, so) in grp:
                    if fold2:
                        stage_c2(dst, tok0, TW, so)
                    else:
                        stage_c(dst, tok0, TW, sidx, so)
            self._lc_all = True

    def fin_phase(self, hsrc):
        k, nc = self.k, self.nc
        with Phase(k):
            hs = [k.sb(f"fh{i}", [128, 4, D], F32) for i in range(2)]
            ho = [k.sb(f"fo{i}", [128, 4, D], F32) for i in range(2)]
            junk = k.sb("fjunk", [128, D], BF16)
            ss = [k.sb(f"fss{i}", [128, 4], F32) for i in range(2)]
            for t in range(self.L // 512):
                h, o, s_ = hs[t % 2], ho[t % 2], ss[t % 2]
                rows = slice(t * 512, (t + 1) * 512)
                k.dma(h[:], hsrc[rows, :].rearrange("(s p) d -> p s d", p=128), W=[h])
                for s in range(4):
                    k.act(junk[:], h[:, s, :], AF.Square, R=[h], W=[junk], WP=[s_], accum_out=s_[:, s:s + 1])
                k.act(s_[:], s_[:], AF.Sqrt, R=[s_], W=[s_], scale=1.0 / D, bias=self.epsb[:, 0:1])
                k.op("dve", lambda: nc.vector.reciprocal(out=s_[:], in_=s_[:]), R=[s_], W=[s_])
                for s in range(4):
                    k.stt("dve", o[:, s, :], h[:, s, :], s_[:, s:s + 1], self.fngb[:],
                          ALU.mult, ALU.mult, R=[h, s_, self.fngb], WP=[o])
                k.dma(self.out[rows, :].rearrange("(s p) d -> p s d", p=128), o[:], R=[o], WP=[self.out])


_PROG_CACHE = {}


def kernel(**inputs):
    inp = {k_: np.asarray(v) for k_, v in inputs.items()}
    B, L, _ = inp["x"].shape
    if L not in _PROG_CACHE:
        _PROG_CACHE[L] = Prog(L)
    P = _PROG_CACHE[L]
    sh, per = host_layout(inp, L)
    sh["natab"] = sh["natab"].reshape(2, 5, 128, -1)
    ncores = B
    maps = [dict(sh, **per[i]) for i in range(ncores)]
    res = run_bass_kernel_spmd(P.nc, maps, core_ids=list(range(ncores)))
    out = np.stack([np.asarray(res.results[b]["out"], dtype=np.float32) for b in range(B)], axis=0)
    return out
```
